# Optimizing a Trainium2 kernel written in Bass

```python
import math
import jax, jax.numpy as jnp
from jax import lax
import numpy as np

D_MODEL = 2048
BATCH = 4
SEQ = 2048
DEPTH = 1

MIX_WIDTH = D_MODEL
ATTN_WIDTH = MIX_WIDTH // 2
POOL_WIDTH = MIX_WIDTH - ATTN_WIDTH
N_DIFF_HEADS = 8
DIFF_HEAD_DIM = ATTN_WIDTH // N_DIFF_HEADS // 2
DIFF_V_DIM = 2 * DIFF_HEAD_DIM
POOL_WINDOWS = (2, 4, 8, 16)
N_POOL_GROUPS = len(POOL_WINDOWS)
POOL_GROUP_DIM = POOL_WIDTH // N_POOL_GROUPS
IN_WIDTH = 3 * ATTN_WIDTH + POOL_WIDTH
N_EXPERT_GROUPS = 4
EXPERTS_PER_GROUP = 8
N_EXPERTS = N_EXPERT_GROUPS * EXPERTS_PER_GROUP
TOP_K_FINE = 2
D_FF_EXPERT = D_MODEL // 4
Q_BLOCK = 128
RMS_EPS = 1e-6
NEG_INF = -1e30

kernel_name = "hymba_diffattn_pool_hmoe_block"


def rmsnorm(x, g):
    xf = x.astype(jnp.float32)
    y = xf * lax.rsqrt(jnp.mean(xf * xf, axis=-1, keepdims=True) + RMS_EPS)
    return (y * g.astype(jnp.float32)).astype(x.dtype)


def alibi_slopes(n_heads):
    return jnp.exp2(-8.0 * jnp.arange(1, n_heads + 1, dtype=jnp.float32) / n_heads)


def diff_attention(q, k, v, lam, subln_g, lam_init):
    B, S = q.shape[0], q.shape[1]
    nb = S // Q_BLOCK
    q1 = q[:, :, :, 0].transpose(0, 2, 1, 3)
    q2 = q[:, :, :, 1].transpose(0, 2, 1, 3)
    k1 = k[:, :, :, 0].transpose(0, 2, 1, 3)
    k2 = k[:, :, :, 1].transpose(0, 2, 1, 3)
    vh = v.transpose(0, 2, 1, 3)
    scale = DIFF_HEAD_DIM ** -0.5
    slopes = alibi_slopes(N_DIFF_HEADS)
    key_pos = jnp.arange(S, dtype=jnp.int32)

    def to_blocks(t):
        return t.reshape(B, N_DIFF_HEADS, nb, Q_BLOCK, -1).transpose(2, 0, 1, 3, 4)

    def block(args):
        q1i, q2i, bi = args
        qpos = bi * Q_BLOCK + jnp.arange(Q_BLOCK, dtype=jnp.int32)
        dist = qpos[:, None] - key_pos[None, :]
        bias = jnp.where(dist[None] >= 0,
                         -slopes[:, None, None] * dist[None].astype(jnp.float32),
                         NEG_INF)
        s1 = jnp.einsum('bhqd,bhkd->bhqk', q1i, k1).astype(jnp.float32) * scale + bias
        s2 = jnp.einsum('bhqd,bhkd->bhqk', q2i, k2).astype(jnp.float32) * scale + bias
        a = jax.nn.softmax(s1, axis=-1) - lam * jax.nn.softmax(s2, axis=-1)
        return jnp.einsum('bhqk,bhkv->bhqv', a.astype(vh.dtype), vh)

    out = lax.map(block, (to_blocks(q1), to_blocks(q2), jnp.arange(nb, dtype=jnp.int32)))
    out = out.transpose(1, 2, 0, 3, 4).reshape(B, N_DIFF_HEADS, S, DIFF_V_DIM)
    out = rmsnorm(out, subln_g) * (1.0 - lam_init)
    return out.transpose(0, 2, 1, 3).reshape(B, S, ATTN_WIDTH)


def pool_mixer(u, pool_w, pool_scale):
    B, S = u.shape[0], u.shape[1]
    ug = u.reshape(B, S, N_POOL_GROUPS, POOL_GROUP_DIM)
    pos = jnp.arange(S, dtype=jnp.int32)
    outs = []
    for gi, w in enumerate(POOL_WINDOWS):
        ch = ug[:, :, gi, :].astype(jnp.float32)
        csum = jnp.pad(jnp.cumsum(ch, axis=1), ((0, 0), (1, 0), (0, 0)))
        lag = jnp.pad(csum, ((0, 0), (w - 1, 0), (0, 0)))[:, :S]
        count = jnp.minimum(pos + 1, w).astype(jnp.float32)
        mean = (csum[:, 1:] - lag) / count[None, :, None]
        outs.append(mean - ch)
    pooled = jnp.stack(outs, axis=2).astype(u.dtype)
    mixed = jnp.einsum('bsgc,gce->bsge', pooled, pool_w)
    return mixed.reshape(B, S, POOL_WIDTH) * pool_scale


def hierarchical_moe(xn, w_coarse, b_coarse, w_fine, b_fine, w_gate, w_up, w_down):
    B, S, D = xn.shape
    T = B * S
    xt = xn.reshape(T, D)
    tok = jnp.arange(T, dtype=jnp.int32)
    coarse = jnp.einsum('td,dg->tg', xt, w_coarse).astype(jnp.float32) + b_coarse.astype(jnp.float32)
    gsel = jnp.argmax(coarse, axis=-1)
    p_group = jax.nn.softmax(coarse, axis=-1)[tok, gsel]
    fine = jnp.einsum('td,gde->tge', xt, w_fine).astype(jnp.float32) + b_fine.astype(jnp.float32)[None]
    fine_sel = fine[tok, gsel]
    top_v, top_i = lax.top_k(fine_sel, TOP_K_FINE)
    wts = jax.nn.softmax(top_v, axis=-1) * p_group[:, None]
    eidx = gsel[:, None] * EXPERTS_PER_GROUP + top_i
    combine = jnp.sum(jax.nn.one_hot(eidx, N_EXPERTS, dtype=jnp.float32) * wts[..., None], axis=1)
    combine = combine.astype(xn.dtype)
    y = jnp.zeros((T, D), dtype=xn.dtype)
    for g in range(N_EXPERT_GROUPS):
        sl = slice(g * EXPERTS_PER_GROUP, (g + 1) * EXPERTS_PER_GROUP)
        hg = jnp.einsum('td,edf->tef', xt, w_gate[sl])
        hu = jnp.einsum('td,edf->tef', xt, w_up[sl])
        h = jax.nn.silu(hg) * hu * combine[:, sl, None]
        y = y + jnp.einsum('tef,efd->td', h, w_down[sl])
    return y.reshape(B, S, D)


def setup_inputs(seed: int = 0) -> dict:
    key = jax.random.key(seed)
    ks = jax.random.split(key, 20)
    f32 = jnp.float32
    L, D = DEPTH, D_MODEL

    def nrm(k, shape, scale):
        return jax.random.normal(k, shape, f32) * scale

    return {
        "x": jax.random.normal(ks[0], (BATCH, SEQ, D), f32),
        "norm1_g": 1.0 + nrm(ks[1], (L, D), 0.02),
        "w_in": nrm(ks[2], (L, D, IN_WIDTH), D ** -0.5),
        "lambda_q1": nrm(ks[3], (L, DIFF_HEAD_DIM), 0.1),
        "lambda_k1": nrm(ks[4], (L, DIFF_HEAD_DIM), 0.1),
        "lambda_q2": nrm(ks[5], (L, DIFF_HEAD_DIM), 0.1),
        "lambda_k2": nrm(ks[6], (L, DIFF_HEAD_DIM), 0.1),
        "subln_g": 1.0 + nrm(ks[7], (L, DIFF_V_DIM), 0.02),
        "pool_w": nrm(ks[8], (L, N_POOL_GROUPS, POOL_GROUP_DIM, POOL_GROUP_DIM), POOL_GROUP_DIM ** -0.5),
        "pool_scale": 1.0 + nrm(ks[9], (L, POOL_WIDTH), 0.1),
        "w_out": nrm(ks[10], (L, MIX_WIDTH, D), MIX_WIDTH ** -0.5),
        "norm2_g": 1.0 + nrm(ks[11], (L, D), 0.02),
        "w_coarse": nrm(ks[12], (L, D, N_EXPERT_GROUPS), D ** -0.5),
        "b_coarse": nrm(ks[13], (L, N_EXPERT_GROUPS), 0.01),
        "w_fine": nrm(ks[14], (L, N_EXPERT_GROUPS, D, EXPERTS_PER_GROUP), D ** -0.5),
        "b_fine": nrm(ks[15], (L, N_EXPERT_GROUPS, EXPERTS_PER_GROUP), 0.01),
        "w_gate": nrm(ks[16], (L, N_EXPERTS, D, D_FF_EXPERT), D ** -0.5),
        "w_up": nrm(ks[17], (L, N_EXPERTS, D, D_FF_EXPERT), D ** -0.5),
        "w_down": nrm(ks[18], (L, N_EXPERTS, D_FF_EXPERT, D), D_FF_EXPERT ** -0.5),
        "final_norm_g": 1.0 + nrm(ks[19], (D,), 0.02),
    }


def reference(x, norm1_g, w_in, lambda_q1, lambda_k1, lambda_q2, lambda_k2, subln_g,
              pool_w, pool_scale, w_out, norm2_g, w_coarse, b_coarse, w_fine, b_fine,
              w_gate, w_up, w_down, final_norm_g):
    B, S, D = x.shape
    h = x
    for l in range(DEPTH):
        lam_init = 0.8 - 0.6 * math.exp(-0.3 * l)
        xn = rmsnorm(h, norm1_g[l])
        proj = jnp.einsum('bsd,de->bse', xn, w_in[l])
        q = proj[..., :ATTN_WIDTH].reshape(B, S, N_DIFF_HEADS, 2, DIFF_HEAD_DIM)
        k = proj[..., ATTN_WIDTH:2 * ATTN_WIDTH].reshape(B, S, N_DIFF_HEADS, 2, DIFF_HEAD_DIM)
        v = proj[..., 2 * ATTN_WIDTH:3 * ATTN_WIDTH].reshape(B, S, N_DIFF_HEADS, DIFF_V_DIM)
        u = proj[..., 3 * ATTN_WIDTH:]
        lam = (jnp.exp(jnp.sum(lambda_q1[l].astype(jnp.float32) * lambda_k1[l].astype(jnp.float32)))
               - jnp.exp(jnp.sum(lambda_q2[l].astype(jnp.float32) * lambda_k2[l].astype(jnp.float32)))
               + lam_init)
        attn_out = diff_attention(q, k, v, lam, subln_g[l], lam_init)
        pool_out = pool_mixer(u, pool_w[l], pool_scale[l])
        mixed = jnp.concatenate([attn_out, pool_out], axis=-1)
        h = h + jnp.einsum('bsm,md->bsd', mixed, w_out[l])
        hn = rmsnorm(h, norm2_g[l])
        h = h + hierarchical_moe(hn, w_coarse[l], b_coarse[l], w_fine[l], b_fine[l],
                                 w_gate[l], w_up[l], w_down[l])
    return rmsnorm(h, final_norm_g)
```

```python
import math
import os
from contextlib import ExitStack

import numpy as np
import concourse.bass as bass
import concourse.mybir as mybir
from concourse.bass_utils import run_bass_kernel_spmd

F32 = mybir.dt.float32
BF16 = mybir.dt.bfloat16
AF = mybir.ActivationFunctionType
ALU = mybir.AluOpType
AX = mybir.AxisListType

D = 2048
DC = 16
S = 2048
NCORES = 8
NEXP = 32
DFF = 512
A_BLOCKS = [0, 3, 4, 7, 8, 11, 12, 15]
B_BLOCKS = [1, 2, 5, 6, 9, 10, 13, 14]
NEG = -30000.0
EPS = 1e-6
N_DMA_SEMS = 24


class Sched:
    ENGS = ("pe", "act", "dve", "pool", "sp")

    def __init__(self, nc, esems, dsems):
        self.nc = nc
        self.eng = {"pe": nc.tensor, "act": nc.scalar, "dve": nc.vector,
                    "pool": nc.gpsimd, "sp": nc.sync}
        self.esem = esems
        self.dsem = dsems
        self.ops = {e: [] for e in self.ENGS}
        self.cnt = {e: 0 for e in self.ENGS}
        self.dcnt = [0] * len(dsems)
        self.waited = {e: {} for e in self.ENGS}
        self.lastw = {}
        self.readers = {}
        self.dma_rr = 0
        self.dma_rr_sw = 0
        self.pending = {e: [] for e in self.ENGS}
        self.enabled = True

    def _sem(self, key):
        return self.esem[key] if isinstance(key, str) else self.dsem[key]

    def _collect(self, eng, reads, writes, is_dma):
        need = {}

        def add(ev):
            if ev is None:
                return
            k, v = ev
            if need.get(k, 0) < v:
                need[k] = v
        for b in reads:
            add(self.lastw.get(b))
        for b in writes:
            add(self.lastw.get(b))
            for ev in self.readers.get(b, ()):
                add(ev)
        out = []
        for k, v in need.items():
            if k == "pe" and eng == "pe" and not is_dma:
                continue
            if self.waited[eng].get(k, 0) >= v:
                continue
            self.waited[eng][k] = v
            out.append((k, v))
        return out

    def _record(self, ev, reads, writes):
        for b in writes:
            self.lastw[b] = ev
            self.readers[b] = []
        for b in reads:
            self.readers.setdefault(b, []).append(ev)

    def op(self, eng, fn, reads=(), writes=(), inc=True):
        if not self.enabled:
            return None
        waits = self._collect(eng, reads, writes, False)
        ev = (eng, self.cnt[eng] + 1)
        if inc:
            self.cnt[eng] += 1
        self._record(ev, reads, writes)
        e = self.eng[eng]
        sem = self.esem[eng]
        wl = [(self._sem(k), v) for k, v in waits]

        def emit():
            for s_, v_ in wl:
                e.wait_ge(s_, v_)
            ins = fn(e)
            if inc:
                ins.then_inc(sem, 1)
        self.ops[eng].append(emit)
        return ev

    def dma(self, q, out, in_, reads=(), writes=()):
        if not self.enabled:
            return None
        waits = self._collect(q, reads, writes, True)
        half = len(self.dsem) // 2
        if q == "pool":
            j = half + self.dma_rr_sw
            self.dma_rr_sw = (self.dma_rr_sw + 1) % half
        else:
            j = self.dma_rr
            self.dma_rr = (self.dma_rr + 1) % half
        self.dcnt[j] += 16
        ev = (j, self.dcnt[j])
        self._record(ev, reads, writes)
        e = self.eng[q]
        sem = self.dsem[j]
        wl = [(self._sem(k), v) for k, v in waits]

        def emit():
            for s_, v_ in wl:
                e.wait_ge(s_, v_)
            e.dma_start(out=out, in_=in_).then_inc(sem, 16)
        self.ops[q].append(emit)
        return ev

    def wait_all(self, eng, events):
        wl = []
        for k, v in events:
            if self.waited[eng].get(k, 0) < v:
                self.waited[eng][k] = v
                wl.append((self._sem(k), v))
        e = self.eng[eng]

        def emit():
            for s_, v_ in wl:
                e.wait_ge(s_, v_)
        self.ops[eng].append(emit)

    def barrier(self, force=False):
        if not self.enabled and not force:
            return
        evs = [(e, self.cnt[e]) for e in self.ENGS if self.cnt[e] > 0]
        evs += [(j, self.dcnt[j]) for j in range(len(self.dsem)) if self.dcnt[j] > 0]
        for e in self.ENGS:
            self.wait_all(e, evs)
        self.lastw.clear()
        self.readers.clear()


def build_nc(dbg=None):
    nc = bass.Bass("TRN2", target_bir_lowering=False)

    def din(name, shape, dt=F32):
        return nc.dram_tensor(name, list(shape), dt, kind="ExternalInput").ap()

    xall = din("xall", [17 * 128, D])
    xown = din("xown", [1024, D])
    g1 = din("g1", [1, D])
    g2 = din("g2", [1, D])
    g3 = din("g3", [1, D])
    w_in = din("w_in", [D, 4096])
    lamv = din("lamv", [1, 256])
    subg = din("subg", [1, 128])
    pool_w = din("pool_w", [4, 256, 256])
    pool_scale = din("pool_scale", [128, 8])
    w_out = din("w_out", [D, D])
    w_rt = din("w_rt", [D, 36])
    b_rt = din("b_rt", [1, 36])
    w_gate = din("w_gate", [NEXP, D, DFF])
    w_up = din("w_up", [NEXP, D, DFF])
    w_down = din("w_down", [NEXP, DFF, D])
    abias = din("abias", [128, 8 * 16 * 8])
    trimask = din("trimask", [128, 128])
    poolfix = din("poolfix", [128, 4 * 8 * 16])
    ident_in = din("ident", [128, 128])
    iota_in = din("iota", [128, 128])
    ustr_in = din("ustr", [128, 128])
    out_d = nc.dram_tensor("out", [1024, D], F32, kind="ExternalOutput").ap()
    dbg_d = None
    if dbg is not None:
        dbg_d = nc.dram_tensor("dbg", list(dbg), F32, kind="ExternalOutput").ap()

    lam_init = 0.8 - 0.6 * math.exp(-0.3 * 0)

    with ExitStack() as es:
        esems = {e: es.enter_context(nc.semaphore("s_" + e)) for e in Sched.ENGS}
        dsems = [es.enter_context(nc.semaphore("d%d" % i)) for i in range(N_DMA_SEMS)]
        P = Sched(nc, esems, dsems)

        ARENA_K = 184
        arena = nc.alloc_sbuf_tensor("arena", [128, ARENA_K * 512], BF16)
        cst = nc.alloc_sbuf_tensor("cst", [128, 5120], F32)
        pall = nc.alloc_psum_tensor("pall", [128, 4096], F32)
        psf = [pall[:, i * 512:(i + 1) * 512] for i in range(7)]
        psb = pall[:, 7 * 512:8 * 512].bitcast(BF16)

        def A16(off_k, n):
            o = int(off_k * 512)
            return arena[:, o:o + n]

        def A32(off_k, n):
            o = int(off_k * 512)
            return arena[:, o:o + 2 * n].bitcast(F32)

        C = cst[:]
        c_ident = C[:, 0:64].bitcast(BF16)
        c_tri = C[:, 64:128].bitcast(BF16)
        c_eps = C[:, 128:129]
        c_lam = C[:, 129:130]
        c_nlam = C[:, 130:131]
        c_tmp = C[:, 132:140]
        c_lamv = C[:, 140:396]
        c_subg = C[:, 396:524]
        c_pscale = C[:, 524:532]
        c_brt = C[:, 532:568]
        c_abias = C[:, 568:1592]
        c_pfix = C[:, 1592:2104]
        c_ss = C[:, 2104:2136]
        c_g = C[:, 2136:4184]
        c_ones = C[:, 4184:4185]
        c_rt = C[:, 4200:5120]

        tmpi = A32(115.5, 128)
        P.dma("sp", tmpi, ident_in, writes=["tmpi"])
        P.op("dve", lambda e: e.tensor_copy(out=c_ident, in_=tmpi), reads=["tmpi"], writes=["ident"])
        tmpt = A32(96.25, 128)
        P.dma("sp", tmpt, trimask, writes=["tmpt"])
        P.op("dve", lambda e: e.tensor_copy(out=c_tri, in_=tmpt), reads=["tmpt"], writes=["tri"])
        P.op("dve", lambda e: e.memset(c_eps, EPS), writes=["eps"])
        P.op("dve", lambda e: e.memset(c_ones, 1.0), writes=["ones"])
        P.dma("sp", c_lamv, lamv.partition_broadcast(128), writes=["lamv"])
        P.dma("sp", c_subg, subg.partition_broadcast(128), writes=["subg"])
        P.dma("sp", c_pscale, pool_scale, writes=["pscale"])
        P.dma("sp", c_brt, b_rt.partition_broadcast(128), writes=["brt"])
        P.dma("sp", c_abias, abias, writes=["abias"])
        P.dma("sp", c_pfix, poolfix, writes=["pfix"])
        P.dma("sp", c_g, g1.partition_broadcast(128), writes=["g"])
        P.op("dve", lambda e: e.tensor_tensor(out=c_lamv[:, 0:64], in0=c_lamv[:, 0:64], in1=c_lamv[:, 64:128], op=ALU.mult),
             reads=["lamv"], writes=["lamv"])
        P.op("dve", lambda e: e.tensor_tensor(out=c_lamv[:, 128:192], in0=c_lamv[:, 128:192], in1=c_lamv[:, 192:256], op=ALU.mult),
             reads=["lamv"], writes=["lamv"])
        P.op("dve", lambda e: e.tensor_reduce(out=c_tmp[:, 0:1], in_=c_lamv[:, 0:64], axis=AX.X, op=ALU.add),
             reads=["lamv"], writes=["tmp0"])
        P.op("dve", lambda e: e.tensor_reduce(out=c_tmp[:, 1:2], in_=c_lamv[:, 128:192], axis=AX.X, op=ALU.add),
             reads=["lamv"], writes=["tmp1"])
        P.op("act", lambda e: e.activation(out=c_tmp[:, 2:4], in_=c_tmp[:, 0:2], func=AF.Exp),
             reads=["tmp0", "tmp1"], writes=["tmp2"])
        P.op("dve", lambda e: e.tensor_tensor(out=c_lam, in0=c_tmp[:, 2:3], in1=c_tmp[:, 3:4], op=ALU.subtract),
             reads=["tmp2"], writes=["lam"])
        P.op("dve", lambda e: e.tensor_scalar(out=c_lam, in0=c_lam, scalar1=lam_init, scalar2=None, op0=ALU.add),
             reads=["lam"], writes=["lam"])
        P.op("dve", lambda e: e.tensor_scalar(out=c_nlam, in0=c_lam, scalar1=-1.0, scalar2=None, op0=ALU.mult),
             reads=["lam"], writes=["nlam"])
        P.op("dve", lambda e: e.tensor_scalar(out=c_subg, in0=c_subg, scalar1=(1.0 - lam_init), scalar2=None, op0=ALU.mult),
             reads=["subg"], writes=["subg"])

        QP = A16(0, 8 * 2 * 1024).rearrange("p (h a t) -> p h a t", h=8, a=2)
        KT = A16(32, 8 * 2048).rearrange("p (c t) -> p c t", c=8)
        V = A16(64, 16 * 8 * 129).rearrange("p (c h v) -> p c h v", c=16, h=8)
        UT = A16(97, 8 * 1152).rearrange("p (c t) -> p c t", c=8)
        XNT = A16(116, 16 * 1152).rearrange("p (c t) -> p c t", c=16)
        XS = [A32(152, 2048), A32(82, 2048)]
        XN = [A16(160, 2048), A16(164, 2048)]
        WS = [A16(168, 16 * 256).rearrange("p (c f) -> p c f", c=16),
              A16(176, 16 * 256).rearrange("p (c f) -> p c f", c=16)]
        NWS = 2

        PSB = psb
        PSB2 = pall[:, 6 * 512:7 * 512].bitcast(BF16)
        mm_rr = [0]

        def mm_bank():
            b = mm_rr[0] % 4
            mm_rr[0] += 1
            return b

        def norm_chunk(src_ap, ci, dst_tok0, ntok_dst=None):
            s = ci % 2
            xs, xn = XS[s], XN[s]
            P.dma("sp", xs, src_ap, writes=["xs%d" % s])
            P.op("act", lambda e: e.activation(out=xn, in_=xs, func=AF.Square, accum_out=c_ss[:, s:s + 1]),
                 reads=["xs%d" % s], writes=["xn%d" % s, "ss%d" % s])
            P.op("act", lambda e: e.activation(out=c_ss[:, 2 + s:3 + s], in_=c_ss[:, s:s + 1], func=AF.Sqrt,
                                               bias=c_eps, scale=1.0 / D),
                 reads=["ss%d" % s, "eps"], writes=["rms%d" % s])
            P.op("dve", lambda e: e.reciprocal(out=c_ss[:, 4 + s:5 + s], in_=c_ss[:, 2 + s:3 + s]),
                 reads=["rms%d" % s], writes=["rstd%d" % s])
            P.op("dve", lambda e: e.scalar_tensor_tensor(out=xn, in0=xs, scalar=c_ss[:, 4 + s:5 + s], in1=c_g,
                                                         op0=ALU.mult, op1=ALU.mult),
                 reads=["xs%d" % s, "rstd%d" % s, "g"], writes=["xn%d" % s])
            for k in range(2):
                pb, pk = (PSB, "psb") if k == 0 else (PSB2, "psf6")
                for j in range(8):
                    dc = k * 8 + j
                    P.op("pe", lambda e, dc=dc, j=j, pb=pb: e.transpose(out=pb[:, j * 128:(j + 1) * 128],
                                                                       in_=xn[:, dc * 128:(dc + 1) * 128], identity=c_ident),
                         reads=["xn%d" % s, "ident"], writes=[pk], inc=(j == 7))
                dst = XNT[:, k * 8:(k + 1) * 8, dst_tok0:dst_tok0 + 128]
                eng = "act" if k == 0 else "dve"
                if eng == "act":
                    P.op("act", lambda e, dst=dst, pb=pb: e.activation(out=dst, in_=pb.rearrange("p (c t) -> p c t", c=8), func=AF.Copy),
                         reads=[pk], writes=["xnt"])
                else:
                    P.op("dve", lambda e, dst=dst, pb=pb: e.tensor_copy(out=dst, in_=pb.rearrange("p (c t) -> p c t", c=8)),
                         reads=[pk], writes=["xnt"])

        wslab_i = [0]

        def load_wslab(col0, ncols=256):
            s = wslab_i[0] % NWS
            wslab_i[0] += 1
            src = w_in[:, col0:col0 + ncols].rearrange("(c p) f -> p c f", p=128)
            P.dma("pool", WS[s][:, :, 0:ncols], src, writes=["ws%d" % s])
            return s

        def proj_featmajor(col0, dst, ntok, tok_src0=0, dst_tok0=0, qp_h0=None):
            s = load_wslab(col0)
            for oc in range(2):
                t0 = 0
                while t0 < ntok:
                    n = min(512, ntok - t0)
                    b = mm_bank()
                    ps = psf[b][:, 0:n]
                    for dc in range(DC):
                        P.op("pe", lambda e, dc=dc, oc=oc, t0=t0, n=n, ps=ps: e.matmul(
                            ps, lhsT=WS[s][:, dc, oc * 128:(oc + 1) * 128],
                            rhs=XNT[:, dc, tok_src0 + t0:tok_src0 + t0 + n], start=(dc == 0), stop=(dc == DC - 1)),
                            reads=["ws%d" % s, "xnt"], writes=["psf%d" % b], inc=(dc == DC - 1))
                    if qp_h0 is not None:
                        hq = qp_h0 + oc
                        P.op("act", lambda e, hq=hq, t0=t0, n=n, ps=ps: e.activation(
                            out=QP[0:64, hq, 0, t0:t0 + n], in_=ps[0:64, :], func=AF.Copy),
                            reads=["psf%d" % b], writes=["projout"])
                        P.op("dve", lambda e, hq=hq, t0=t0, n=n, ps=ps: e.tensor_copy(
                            out=QP[64:128, hq, 1, t0:t0 + n], in_=ps[64:128, :]),
                            reads=["psf%d" % b], writes=["projout"])
                        t0 += n
                        continue
                    d_ap = dst[:, oc, dst_tok0 + t0:dst_tok0 + t0 + n]
                    if (mm_rr[0] % 2) == 0:
                        P.op("act", lambda e, d_ap=d_ap, ps=ps: e.activation(out=d_ap, in_=ps, func=AF.Copy),
                             reads=["psf%d" % b], writes=["projout"])
                    else:
                        P.op("dve", lambda e, d_ap=d_ap, ps=ps: e.tensor_copy(out=d_ap, in_=ps),
                             reads=["psf%d" % b], writes=["projout"])
                    t0 += n

        def proj_v(col0, hh, chunk_src0, nchunks, chunk_dst0):
            s = load_wslab(col0)
            for ci in range(nchunks):
                b = mm_bank()
                ps = psf[b][:, 0:256]
                for dc in range(DC):
                    P.op("pe", lambda e, dc=dc, ci=ci, ps=ps: e.matmul(
                        ps, lhsT=XNT[:, dc, (chunk_src0 + ci) * 128:(chunk_src0 + ci + 1) * 128],
                        rhs=WS[s][:, dc, 0:256], start=(dc == 0), stop=(dc == DC - 1)),
                        reads=["ws%d" % s, "xnt"], writes=["psf%d" % b], inc=(dc == DC - 1))
                d_ap = V[:, chunk_dst0 + ci, 2 * hh:2 * hh + 2, 0:128]
                src = ps.rearrange("p (h v) -> p h v", h=2)
                if ci % 2 == 0:
                    P.op("act", lambda e, d_ap=d_ap, src=src: e.activation(out=d_ap, in_=src, func=AF.Copy),
                         reads=["psf%d" % b], writes=["V"])
                else:
                    P.op("dve", lambda e, d_ap=d_ap, src=src: e.tensor_copy(out=d_ap, in_=src),
                         reads=["psf%d" % b], writes=["V"])

        P.op("pool", lambda e: e.memset(V[:, 0:8, :, 128:129], 1.0), writes=["V"])
        P.op("pool", lambda e: e.memset(A16(0, 16384), 0.0), writes=["projout"])
        for ci in range(9):
            src_chunk = ci if ci < 8 else 16
            norm_chunk(xall[src_chunk * 128:(src_chunk + 1) * 128, :], ci, ci * 128)
        for oc2 in range(4):
            proj_featmajor(oc2 * 256, None, 1024, qp_h0=2 * oc2)
        for oc2 in range(4):
            proj_featmajor(1024 + oc2 * 256, KT[:, 2 * oc2:2 * oc2 + 2, :], 1024)
        for hh in range(4):
            proj_v(2048 + hh * 256, hh, 0, 8, 0)
        UTv = UT.rearrange("p c (b t) -> p c b t", b=8)
        for oc2 in range(4):
            s = load_wslab(3072 + oc2 * 256)
            for oc in range(2):
                for half in range(3):
                    t0 = half * 512
                    n = 512 if half < 2 else 128
                    b = mm_bank()
                    ps = psf[b][:, 0:n]
                    for dc in range(DC):
                        P.op("pe", lambda e, dc=dc, oc=oc, t0=t0, n=n, ps=ps, s=s: e.matmul(
                            ps, lhsT=WS[s][:, dc, oc * 128:(oc + 1) * 128], rhs=XNT[:, dc, t0:t0 + n],
                            start=(dc == 0), stop=(dc == DC - 1)),
                            reads=["ws%d" % s, "xnt"], writes=["psf%d" % b], inc=(dc == DC - 1))
                    if half < 2:
                        d_ap = UTv[:, 2 * oc2 + oc, half * 4:half * 4 + 4, 16:144]
                        src = ps.rearrange("p (b t) -> p b t", b=4)
                    else:
                        d_ap = UTv[:, 2 * oc2 + oc, :, 0:16]
                        src = ps.rearrange("p (b t) -> p b t", b=8)
                    P.op("dve", lambda e, d_ap=d_ap, src=src: e.tensor_copy(out=d_ap, in_=src),
                         reads=["psf%d" % b], writes=["UT"])
        for ci in range(8):
            norm_chunk(xall[(8 + ci) * 128:(9 + ci) * 128, :], ci, ci * 128)
        P.op("pool", lambda e: e.memset(V[:, 8:16, :, 128:129], 1.0), writes=["V", "xs1"])
        for oc2 in range(4):
            proj_featmajor(1024 + oc2 * 256, KT[:, 2 * oc2:2 * oc2 + 2, :], 1024, dst_tok0=1024)
        for hh in range(4):
            proj_v(2048 + hh * 256, hh, 0, 8, 8)


        P.barrier()
        MT = A16(116, 16 * 1024).rearrange("p (c t) -> p c t", c=16)
        ET = [A16(148 + 0.5 * r, 256) for r in range(4)]
        O1 = [A32(150 + 0.5 * r, 128) for r in range(2)]
        O2 = [A32(151 + 0.5 * r, 128) for r in range(2)]
        AO = A16(152, 2 * 1024).rearrange("p (b f) -> p b f", b=2)
        TB = [A32(156 + 4.5 * r, 1152).rearrange("p (b t) -> p b t", b=8) for r in range(3)]
        PT = [A16(170 + 4 * r, 2048).rearrange("p (c t) -> p c t", c=2) for r in range(2)]
        PW = A16(178, 4 * 2 * 256).rearrange("p (g c e) -> p g c e", g=4, c=2)
        c_st = C[:, 4200:4300]

        P.dma("pool", PW, pool_w.rearrange("g (c p) e -> p g c e", p=128), writes=["PW"])

        if KPART == 'attn':
            P.enabled = False
        UTv2 = UT.rearrange("p c (b t) -> p c b t", b=8)
        for c in range(8):
            g = c // 2
            w = 2 << g
            u = UTv2[:, c]
            cur = u
            sh = 1
            k = 0
            while sh < w:
                dst = TB[k % 2]
                P.op("dve", lambda e, dst=dst, cur=cur, sh=sh: e.tensor_tensor(
                    out=dst[:, :, sh:144], in0=cur[:, :, sh:144], in1=cur[:, :, 0:144 - sh], op=ALU.add),
                    reads=["UT", "TB%d" % ((k + 1) % 2)], writes=["TB%d" % (k % 2)])
                if sh > 1 or True:
                    pass
                cur = dst
                sh *= 2
                k += 1
            last = (k - 1) % 2
            t2 = TB[2]
            P.op("dve", lambda e, cur=cur, w=w, t2=t2: e.tensor_scalar(
                out=t2[:, :, 0:128], in0=cur[:, :, 16:144], scalar1=1.0 / w, scalar2=None, op0=ALU.mult),
                reads=["TB%d" % last], writes=["TB2"])
            pf = c_pfix.rearrange("p (g b t) -> p g b t", g=4, b=8)[:, g]
            P.op("dve", lambda e, t2=t2, pf=pf: e.tensor_tensor(
                out=t2[:, :, 0:16], in0=t2[:, :, 0:16], in1=pf, op=ALU.mult),
                reads=["TB2", "pfix"], writes=["TB2"])
            ptd = PT[g % 2][:, c % 2, :].rearrange("p (b t) -> p b t", b=8)
            P.op("dve", lambda e, t2=t2, u=u, ptd=ptd: e.tensor_tensor(
                out=ptd, in0=t2[:, :, 0:128], in1=u[:, :, 16:144], op=ALU.subtract),
                reads=["TB2", "UT"], writes=["PT%d_%d" % (g % 2, c % 2)])
            if c % 2 == 1:
                for ec in range(2):
                    for th in range(2):
                        b = 6
                        ps = psf[b][:, 0:512]
                        for cc in range(2):
                            P.op("pe", lambda e, g=g, cc=cc, ec=ec, th=th, ps=ps: e.matmul(
                                ps, lhsT=PW[:, g, cc, ec * 128:(ec + 1) * 128],
                                rhs=PT[g % 2][:, cc, th * 512:(th + 1) * 512], start=(cc == 0), stop=(cc == 1)),
                                reads=["PW", "PT%d_0" % (g % 2), "PT%d_1" % (g % 2)], writes=["psf%d" % b], inc=(cc == 1))
                        P.op("act", lambda e, g=g, ec=ec, th=th, ps=ps: e.activation(
                            out=MT[:, 8 + 2 * g + ec, th * 512:(th + 1) * 512], in_=ps, func=AF.Copy,
                            scale=c_pscale[:, 2 * g + ec:2 * g + ec + 1]),
                            reads=["psf%d" % b, "pscale"], writes=["MT"])

        P.enabled = (KPART != 'pool')
        ABv = c_abias.rearrange("p (i j h) -> p i j h", i=8, j=16)
        SB = [psf[4][:, 0:256], psf[5][:, 0:256], psf[6][:, 0:256]]
        ACC = [(psf[0], psf[1]), (psf[2], psf[3])]
        items = []
        for i in range(8):
            for h in range(8):
                js_list = list(range(i + 1)) + [8 + j for j in range(i + 1)]
                for n_, js in enumerate(js_list):
                    items.append((i, h, js, n_ == 0, n_ == len(js_list) - 1))

        def emit_S(n):
            i, h, js, first, last = items[n]
            sb = SB[n % 3]
            P.op("pe", lambda e: e.matmul(sb.rearrange("p (a q) -> p a q", a=2), lhsT=KT[:, h, js * 128:(js + 1) * 128],
                                          rhs=QP[:, h, :, i * 128:(i + 1) * 128], start=True, stop=True),
                 reads=["KT", "projout"], writes=["psf%d" % (4 + n % 3)], inc=True)

        gi_ = [0]

        def emit_rest(n):
            i, h, js, first, last = items[n]
            sb = SB[n % 3]
            et = ET[n % 4]
            bias = ABv[:, i, js, h:h + 1]
            P.op("act", lambda e: e.activation(out=et, in_=sb, func=AF.Exp, bias=bias, scale=0.125),
                 reads=["psf%d" % (4 + n % 3), "abias"], writes=["E%d" % (n % 4)])
            if js == i:
                et3 = et.rearrange("p (a q) -> p a q", a=2)
                tri3 = c_tri.unsqueeze(1).broadcast_to([128, 2, 128])
                P.op("dve", lambda e: e.tensor_tensor(out=et3, in0=et3, in1=tri3, op=ALU.mult),
                     reads=["E%d" % (n % 4), "tri"], writes=["E%d" % (n % 4)])
            gi = gi_[0]
            a1, a2 = ACC[gi % 2]
            P.op("pe", lambda e: e.matmul(a1[:, 0:129], lhsT=et[:, 0:128], rhs=V[:, js, h, :], start=first, stop=last),
                 reads=["E%d" % (n % 4), "V"], writes=["acc%d" % (gi % 2)], inc=False)
            P.op("pe", lambda e: e.matmul(a2[:, 0:129], lhsT=et[:, 128:256], rhs=V[:, js, h, :], start=first, stop=last),
                 reads=["E%d" % (n % 4), "V"], writes=["acc%d" % (gi % 2)], inc=True)
            if last:
                fin_q.extend([(gi, f_) for f_ in make_finalize(i, h, gi % 2, a1, a2)])
                gi_[0] += 1

        fin_q = []

        def make_finalize(i, h, r, a1, a2):
            st = c_st[:, 10 * r:10 * r + 10]
            ak = "acc%d" % r
            o1, o2 = O1[r], O2[r]

            def d1():
                P.op("dve", lambda e: e.reciprocal(out=st[:, 0:1], in_=a1[:, 128:129]), reads=[ak], writes=["st%d" % r])
                P.op("dve", lambda e: e.reciprocal(out=st[:, 1:2], in_=a2[:, 128:129]), reads=[ak], writes=["st%d" % r])
                P.op("dve", lambda e: e.tensor_tensor(out=st[:, 2:3], in0=st[:, 1:2], in1=c_nlam, op=ALU.mult),
                     reads=["st%d" % r, "nlam"], writes=["st%d" % r])

            def a1s():
                P.op("dve", lambda e: e.tensor_scalar(out=o1, in0=a1[:, 0:128], scalar1=st[:, 0:1], scalar2=None, op0=ALU.mult),
                     reads=[ak, "st%d" % r], writes=["o1_%d" % r])

            def d2():
                P.op("dve", lambda e: e.scalar_tensor_tensor(out=o2, in0=a2[:, 0:128], scalar=st[:, 2:3], in1=o1,
                                                             op0=ALU.mult, op1=ALU.add),
                     reads=[ak, "st%d" % r, "o1_%d" % r], writes=["o2_%d" % r])

            def a23():
                P.op("dve", lambda e: e.tensor_tensor(out=o1, in0=o2, in1=o2, op=ALU.mult),
                     reads=["o2_%d" % r], writes=["o1_%d" % r])
                P.op("dve", lambda e: e.tensor_reduce(out=st[:, 3:4], in_=o1, axis=AX.X, op=ALU.add),
                     reads=["o1_%d" % r], writes=["sq%d" % r])
                P.op("act", lambda e: e.activation(out=st[:, 4:5], in_=st[:, 3:4], func=AF.Ln, bias=c_eps, scale=1.0 / 128),
                     reads=["sq%d" % r, "eps"], writes=["rm%d" % r])
                P.op("act", lambda e: e.activation(out=st[:, 5:6], in_=st[:, 4:5], func=AF.Exp, scale=-0.5),
                     reads=["rm%d" % r], writes=["rs%d" % r])

            def d3():
                P.op("dve", lambda e: e.scalar_tensor_tensor(out=AO[:, i % 2, h * 128:(h + 1) * 128], in0=o2, scalar=st[:, 5:6],
                                                             in1=c_subg, op0=ALU.mult, op1=ALU.mult),
                     reads=["o2_%d" % r, "rs%d" % r, "subg"], writes=["AO%d" % (i % 2)])

            def tr():
                for hh in range(8):
                    P.op("pe", lambda e, hh=hh: e.transpose(out=PSB[:, hh * 128:(hh + 1) * 128],
                                                           in_=AO[:, i % 2, hh * 128:(hh + 1) * 128], identity=c_ident),
                         reads=["AO%d" % (i % 2), "ident"], writes=["psb"], inc=(hh == 7))
                P.op("dve", lambda e: e.tensor_copy(out=MT[:, 0:8, i * 128:(i + 1) * 128],
                                                    in_=PSB.rearrange("p (c t) -> p c t", c=8)),
                     reads=["psb"], writes=["MT"])
            stages = [d1, a1s, d2, a23, d3]
            if h == 7:
                stages.append(tr)
            return stages

        NI = len(items)
        emit_S(0)
        if NI > 1:
            emit_S(1)
        for n in range(NI):
            if n + 2 < NI:
                emit_S(n + 2)
            if items[n][3]:
                while fin_q and fin_q[0][0] <= gi_[0] - 2:
                    fin_q.pop(0)[1]()
            emit_rest(n)
            if fin_q:
                fin_q.pop(0)[1]()
        while fin_q:
            fin_q.pop(0)[1]()

        P.enabled = True
        if DBG_STAGE == 2:
            P.barrier()
            stg = A32(156, 1024)
            for c in range(16):
                P.op("dve", lambda e, c=c: e.tensor_copy(out=stg, in_=MT[:, c, :]), reads=[], writes=["stg"])
                P.dma("sp", dbg_d[c * 128:(c + 1) * 128, 0:1024], stg, reads=["stg"], writes=["dbgout"])
            P.barrier()
            P.enabled = False
        P.barrier()
        H = A32(0, 8 * 2048).rearrange("p (c d) -> p c d", c=8)
        HNT = A16(116, 16 * 1024).rearrange("p (c t) -> p c t", c=16)
        HNK = A16(64, 8 * 2048).rearrange("p (c d) -> p c d", c=8)
        WO = [A16(148 + 16 * r, 16 * 512).rearrange("p (c f) -> p c f", c=16) for r in range(2)]
        JUNK2 = A16(108, 2048)
        for tc in range(8):
            P.dma("sp", H[:, tc, :], xown[tc * 128:(tc + 1) * 128, :], writes=["H%d" % tc])
        P.dma("sp", c_g, g2.partition_broadcast(128), writes=["g"])
        for ds in range(4):
            s = ds % 2
            P.dma("pool", WO[s], w_out[:, ds * 512:(ds + 1) * 512].rearrange("(c p) f -> p c f", p=128), writes=["wo%d" % s])
            for tc in range(8):
                b = mm_bank()
                ps = psf[b][:, 0:512]
                for fc in range(16):
                    P.op("pe", lambda e, fc=fc, tc=tc, s=s, ps=ps: e.matmul(
                        ps, lhsT=MT[:, fc, tc * 128:(tc + 1) * 128], rhs=WO[s][:, fc, :], start=(fc == 0), stop=(fc == 15)),
                        reads=["MT", "wo%d" % s], writes=["psf%d" % b], inc=(fc == 15))
                hs = H[:, tc, ds * 512:(ds + 1) * 512]
                P.op("dve", lambda e, hs=hs, ps=ps: e.tensor_tensor(out=hs, in0=hs, in1=ps, op=ALU.add),
                     reads=["psf%d" % b, "H%d" % tc], writes=["H%d" % tc])

        def norm_tok(tc, src, gkey, dst_bf16=None, dst_f32=None, junk=None):
            s = tc % 2
            st = c_st[:, 30 + 4 * s:34 + 4 * s]
            P.op("act", lambda e: e.activation(out=junk, in_=src, func=AF.Square, accum_out=st[:, 0:1]),
                 reads=["H%d" % tc], writes=["junk2", "n_ss%d" % s])
            P.op("act", lambda e: e.activation(out=st[:, 1:2], in_=st[:, 0:1], func=AF.Sqrt, bias=c_eps, scale=1.0 / D),
                 reads=["n_ss%d" % s, "eps"], writes=["n_rms%d" % s])
            P.op("dve", lambda e: e.reciprocal(out=st[:, 2:3], in_=st[:, 1:2]), reads=["n_rms%d" % s], writes=["n_rstd%d" % s])
            return st[:, 2:3], "n_rstd%d" % s

        P.barrier()
        for tc in range(8):
            s = tc % 2
            rstd, rk = norm_tok(tc, H[:, tc, :], "g", junk=JUNK2)
            hn = HNK[:, tc, :]
            P.op("dve", lambda e, hn=hn, tc=tc, rstd=rstd: e.scalar_tensor_tensor(
                out=hn, in0=H[:, tc, :], scalar=rstd, in1=c_g, op0=ALU.mult, op1=ALU.mult),
                reads=["H%d" % tc, rk, "g"], writes=["hnk%d" % tc])
            for k in range(2):
                pb, pk = (PSB, "psb") if k == 0 else (PSB2, "psf6")
                for j in range(8):
                    dc = k * 8 + j
                    P.op("pe", lambda e, dc=dc, j=j, hn=hn, pb=pb: e.transpose(out=pb[:, j * 128:(j + 1) * 128],
                                                                              in_=hn[:, dc * 128:(dc + 1) * 128], identity=c_ident),
                         reads=["hnk%d" % tc, "ident"], writes=[pk], inc=(j == 7))
                dst = HNT[:, k * 8:(k + 1) * 8, tc * 128:(tc + 1) * 128]
                if k == 0:
                    P.op("act", lambda e, dst=dst, pb=pb: e.activation(out=dst, in_=pb.rearrange("p (c t) -> p c t", c=8), func=AF.Copy),
                         reads=[pk], writes=["HNT"])
                else:
                    P.op("dve", lambda e, dst=dst, pb=pb: e.tensor_copy(out=dst, in_=pb.rearrange("p (c t) -> p c t", c=8)),
                         reads=[pk], writes=["HNT"])

        if DBG_STAGE == 3:
            P.barrier()
            for tc in range(8):
                P.dma("sp", dbg_d[tc * 128:(tc + 1) * 128, 0:2048], H[:, tc, :], reads=[], writes=["dbgout"])
            stg = A32(100, 1024)
            for c in range(16):
                P.op("dve", lambda e, c=c: e.tensor_copy(out=stg, in_=HNT[:, c, :]), reads=[], writes=["stg"])
                P.dma("sp", dbg_d[1024 + c * 128:1024 + (c + 1) * 128, 0:1024], stg, reads=["stg"], writes=["dbgout"])
            P.barrier()
            P.enabled = False
        P.barrier()
        NUNIT = 7
        WU_ = [A16(116 + 8 * r, 4096) for r in range(NUNIT)]
        XGB = [A16(96 + 4 * r, 16 * 128).rearrange("p (c r) -> p c r", c=16) for r in range(2)]
        YG = A16(104, 2 * 2048).rearrange("p (e d) -> p e d", e=2)
        SELB = [A16(112 + 2 * r, 8 * 128).rearrange("p (c j) -> p c j", c=8) for r in range(2)]
        SELT = A16(172, 2 * 1024).rearrange("p (e t) -> p e t", e=2)
        HTE = A16(176, 512).rearrange("p (c r) -> p c r", c=4)
        HROW = A16(177, 512)
        SILT = A32(178, 512)
        RS = A32(180, 1024)
        WR = A16(172, 16 * 36).rearrange("p (c n) -> p c n", c=16)
        P.dma("pool", WR, w_rt.rearrange("(c p) n -> p c n", p=128), writes=["wr"])
        RT2 = C[:, 2136:4184]
        c_iota = RT2[:, 0:128]
        c_ustr = RT2[:, 128:192].bitcast(BF16)
        c_onem = RT2[:, 192:256].bitcast(BF16)
        A_bf = RT2[:, 256:384].bitcast(BF16)
        OFF = RT2[:, 384:640]
        POS = RT2[:, 640:896]
        P.dma("sp", c_iota, iota_in, reads=["g"], writes=["iota"])
        tmpu = A32(179, 128)
        P.dma("sp", tmpu, ustr_in, writes=["tmpu"])
        P.op("dve", lambda e: e.tensor_copy(out=c_ustr, in_=tmpu), reads=["tmpu", "g"], writes=["ustr"])
        P.op("dve", lambda e: e.memset(c_onem, 1.0), reads=["g"], writes=["onem"])
        LGP = psf[6][:, 0:288]
        for tc in range(8):
            for dc in range(16):
                P.op("pe", lambda e, tc=tc, dc=dc: e.matmul(LGP[:, tc * 36:(tc + 1) * 36], lhsT=HNT[:, dc, tc * 128:(tc + 1) * 128],
                                                           rhs=WR[:, dc, :], start=(dc == 0), stop=(dc == 15)),
                     reads=["HNT", "wr"], writes=["psf6", "wu0", "wu1", "wu2", "wu3"], inc=(dc == 15 and tc == 7))
        LG = RS[:, 0:288].rearrange("p (t n) -> p t n", t=8)
        FM = RS[:, 288:544]
        M1 = RS[:, 544:800]
        M2 = c_rt[:, 0:256]
        COMB = c_rt[:, 256:512]
        sm = c_rt[:, 512:900]
        cmax = sm[:, 0:8]
        gmask = sm[:, 8:40].rearrange("p (t g) -> p t g", t=8)
        ecx = sm[:, 40:72].rearrange("p (t g) -> p t g", t=8)
        se = sm[:, 72:80]
        pg = sm[:, 80:88]
        pen = sm[:, 88:120].rearrange("p (t g) -> p t g", t=8)
        m1 = sm[:, 120:128]
        m2 = sm[:, 128:136]
        dd = sm[:, 136:144]
        ee = sm[:, 144:152]
        w1 = sm[:, 152:160]
        w2 = sm[:, 160:168]
        BIG = 1.0e9
        rk = ["rt"]

        def R(fn, eng="dve"):
            P.op(eng, fn, reads=rk, writes=rk)
        P.op("dve", lambda e: e.tensor_tensor(out=LG, in0=LGP.rearrange("p (t n) -> p t n", t=8),
                                              in1=c_brt.unsqueeze(1).broadcast_to([128, 8, 36]), op=ALU.add),
             reads=["psf6", "brt"], writes=rk)
        coarse = LG[:, :, 0:4]
        fine = LG[:, :, 4:36].rearrange("p t (g x) -> p t g x", g=4)
        R(lambda e: e.tensor_reduce(out=cmax, in_=coarse, axis=AX.X, op=ALU.max))
        R(lambda e: e.tensor_tensor(out=gmask, in0=coarse, in1=cmax.unsqueeze(2).broadcast_to([128, 8, 4]), op=ALU.is_ge))
        R(lambda e: e.tensor_tensor(out=ecx, in0=coarse, in1=cmax.unsqueeze(2).broadcast_to([128, 8, 4]), op=ALU.subtract))
        R(lambda e: e.activation(out=ecx, in_=ecx, func=AF.Exp), "act")
        R(lambda e: e.tensor_reduce(out=se, in_=ecx, axis=AX.X, op=ALU.add))
        R(lambda e: e.reciprocal(out=pg, in_=se))
        R(lambda e: e.tensor_scalar(out=pen, in0=gmask, scalar1=BIG, scalar2=-BIG, op0=ALU.mult, op1=ALU.add))
        FM4 = FM.rearrange("p (t g x) -> p t g x", t=8, g=4)
        R(lambda e: e.tensor_tensor(out=FM4, in0=fine, in1=pen.unsqueeze(3).broadcast_to([128, 8, 4, 8]), op=ALU.add))
        FM3 = FM.rearrange("p (t n) -> p t n", t=8)
        M13 = M1.rearrange("p (t n) -> p t n", t=8)
        M23 = M2.rearrange("p (t n) -> p t n", t=8)
        CB3 = COMB.rearrange("p (t n) -> p t n", t=8)
        R(lambda e: e.tensor_reduce(out=m1, in_=FM3, axis=AX.X, op=ALU.max))
        R(lambda e: e.tensor_tensor(out=M13, in0=FM3, in1=m1.unsqueeze(2).broadcast_to([128, 8, 32]), op=ALU.is_ge))
        R(lambda e: e.scalar_tensor_tensor(out=FM, in0=M1, scalar=-BIG, in1=FM, op0=ALU.mult, op1=ALU.add))
        R(lambda e: e.tensor_reduce(out=m2, in_=FM3, axis=AX.X, op=ALU.max))
        R(lambda e: e.tensor_tensor(out=M23, in0=FM3, in1=m2.unsqueeze(2).broadcast_to([128, 8, 32]), op=ALU.is_ge))
        R(lambda e: e.tensor_tensor(out=dd, in0=m2, in1=m1, op=ALU.subtract))
        R(lambda e: e.activation(out=ee, in_=dd, func=AF.Exp), "act")
        R(lambda e: e.tensor_scalar(out=dd, in0=ee, scalar1=1.0, scalar2=None, op0=ALU.add))
        R(lambda e: e.reciprocal(out=w1, in_=dd))
        R(lambda e: e.tensor_tensor(out=w2, in0=ee, in1=w1, op=ALU.mult))
        R(lambda e: e.tensor_tensor(out=w1, in0=w1, in1=pg, op=ALU.mult))
        R(lambda e: e.tensor_tensor(out=w2, in0=w2, in1=pg, op=ALU.mult))
        R(lambda e: e.tensor_tensor(out=M13, in0=M13, in1=w1.unsqueeze(2).broadcast_to([128, 8, 32]), op=ALU.mult))
        R(lambda e: e.tensor_tensor(out=M23, in0=M23, in1=w2.unsqueeze(2).broadcast_to([128, 8, 32]), op=ALU.mult))
        R(lambda e: e.tensor_tensor(out=COMB, in0=M1, in1=M2, op=ALU.add))

        if DBG_STAGE == 4:
            P.barrier()
            P.dma("sp", dbg_d[0:128, 0:256], COMB, reads=[], writes=["dbgout"])
            P.dma("sp", dbg_d[128:256, 0:288], RS[:, 0:288], reads=[], writes=["dbgout"])
            P.barrier()
            P.enabled = False
        POS3 = POS.rearrange("p (c e) -> p c e", c=8)
        OFF3 = OFF.rearrange("p (c e) -> p c e", c=8)
        P.op("dve", lambda e: e.tensor_scalar(out=A_bf, in0=COMB, scalar1=0.0, scalar2=None, op0=ALU.is_gt),
             reads=rk + ["g"], writes=["abf"])
        P.op("pe", lambda e: e.matmul(psf[0][:, 0:256], lhsT=c_ustr, rhs=A_bf, start=True, stop=True),
             reads=["abf", "ustr"], writes=["psf0"])
        P.op("pe", lambda e: e.matmul(psf[1][:, 0:256], lhsT=c_onem, rhs=A_bf, start=True, stop=True),
             reads=["abf", "onem"], writes=["psf1"])
        TOT3 = psf[1][:, 0:256].rearrange("p (c e) -> p c e", c=8)
        P.op("dve", lambda e: e.memset(OFF3[:, 0, :], 0.0), reads=["g"], writes=["off"])
        for c in range(1, 8):
            P.op("dve", lambda e, c=c: e.tensor_tensor(out=OFF3[:, c, :], in0=OFF3[:, c - 1, :], in1=TOT3[:, c - 1, :], op=ALU.add),
                 reads=["psf1", "off"], writes=["off"])
        P.op("dve", lambda e: e.tensor_tensor(out=POS, in0=OFF, in1=psf[0][:, 0:256], op=ALU.add),
             reads=["psf0", "off"], writes=["pos"])
        P.op("dve", lambda e: e.tensor_scalar(out=POS, in0=POS, scalar1=1.0, scalar2=None, op0=ALU.add), reads=["pos"], writes=["pos"])
        P.op("dve", lambda e: e.tensor_tensor(out=POS, in0=POS, in1=A_bf, op=ALU.mult), reads=["pos", "abf"], writes=["pos"])
        P.op("dve", lambda e: e.tensor_scalar(out=POS, in0=POS, scalar1=-1.0, scalar2=None, op0=ALU.add), reads=["pos"], writes=["pos"])

        poolA = [0, 1, 6]
        pa_i = [0]

        def bankA():
            b = poolA[pa_i[0] % 3]
            pa_i[0] += 1
            return b
        sc_i = [0]
        un_i = [0]
        iota3 = c_iota.unsqueeze(1).broadcast_to([128, 8, 128])

        def load_unit(src, view, kw):
            k = un_i[0] % NUNIT
            un_i[0] += 1
            dst = WU_[k].rearrange(view, **kw)
            P.dma("pool", dst, src, writes=["wu%d" % k])
            return dst, "wu%d" % k

        def prep_sel(ex):
            sl = SELB[ex % 2]
            P.op("dve", lambda e: e.tensor_tensor(
                out=sl, in0=iota3, in1=POS3[:, :, ex:ex + 1].broadcast_to([128, 8, 128]), op=ALU.is_equal),
                reads=["pos", "iota"], writes=["sel%d" % (ex % 2)])

        def prep_gather(ex, half):
            sl = SELB[ex % 2]
            xg = XGB[ex % 2]
            for q4 in range(2 * half, 2 * half + 2):
                b = bankA()
                ps = psf[b]
                for d4 in range(4):
                    dc = 4 * q4 + d4
                    for c in range(8):
                        P.op("pe", lambda e, ps=ps, d4=d4, dc=dc, c=c: e.matmul(
                            ps[:, d4 * 128:(d4 + 1) * 128], lhsT=HNK[:, c, dc * 128:(dc + 1) * 128],
                            rhs=sl[:, c, :], start=(c == 0), stop=(c == 7)),
                            reads=["hnk%d" % c, "sel%d" % (ex % 2)], writes=["psf%d" % b], inc=(c == 7 and d4 == 3))
                P.op("act", lambda e, ps=ps, q4=q4: e.activation(
                    out=xg[:, 4 * q4:4 * q4 + 4, :], in_=ps.rearrange("p (a r) -> p a r", a=4), func=AF.Copy),
                    reads=["psf%d" % b], writes=["xg%d" % (ex % 2)])

        def prep_selt(ex):
            sl = SELB[ex % 2]
            P.op("dve", lambda e: e.tensor_tensor(
                out=sl, in0=sl, in1=CB3[:, :, ex:ex + 1].broadcast_to([128, 8, 128]), op=ALU.mult),
                reads=["sel%d" % (ex % 2)] + rk, writes=["sel%d" % (ex % 2)])
            for c in range(8):
                P.op("pe", lambda e, c=c: e.transpose(out=PSB[:, c * 128:(c + 1) * 128], in_=sl[:, c, :], identity=c_ident),
                     reads=["sel%d" % (ex % 2), "ident"], writes=["psb"], inc=(c == 7))
            P.op("act", lambda e: e.activation(out=SELT[:, ex % 2, :], in_=PSB, func=AF.Copy),
                 reads=["psb"], writes=["selt%d" % (ex % 2)])

        unit_tab = {}
        ld_state = {"next": 0, "consumed": -1}

        def unit_src(u):
            ex, j = divmod(u, 6)
            hh = j % 2
            if j < 2:
                return (w_gate[ex][hh * 1024:(hh + 1) * 1024, :].rearrange("(c p) f -> p c f", p=128), "p (c f) -> p c f", dict(c=8))
            if j < 4:
                return (w_up[ex][hh * 1024:(hh + 1) * 1024, :].rearrange("(c p) f -> p c f", p=128), "p (c f) -> p c f", dict(c=8))
            return (w_down[ex][hh * 256:(hh + 1) * 256, :].rearrange("(c p) d -> p c d", p=128), "p (c d) -> p c d", dict(c=2))

        def pump():
            while ld_state["next"] < 6 * NEXP and ld_state["next"] - NUNIT <= ld_state["consumed"]:
                u = ld_state["next"]
                src, view, kw = unit_src(u)
                unit_tab[u] = load_unit(src, view, kw)
                ld_state["next"] += 1

        def units_of(ex):
            return [unit_tab[6 * ex + j] for j in range(6)]

        def exp_A(ex, units):
            xg = XGB[ex % 2]
            for m, pb in ((0, 2), (1, 3)):
                for dc in range(16):
                    wv, wk = units[2 * m + dc // 8]
                    P.op("pe", lambda e, dc=dc, wv=wv, pb=pb: e.matmul(psf[pb], lhsT=xg[:, dc, :], rhs=wv[:, dc % 8, :],
                                                                     start=(dc == 0), stop=(dc == 15)),
                         reads=["xg%d" % (ex % 2), wk], writes=["psf%d" % pb], inc=(dc == 15))
            P.op("act", lambda e: e.activation(out=SILT, in_=psf[2], func=AF.Silu), reads=["psf2"], writes=["silt"])
            P.op("dve", lambda e: e.tensor_tensor(out=HROW, in0=SILT, in1=psf[3], op=ALU.mult),
                 reads=["silt", "psf3"], writes=["hrow"])

        def exp_B(ex):
            for fc in range(4):
                P.op("pe", lambda e, fc=fc: e.transpose(out=PSB[:, fc * 128:(fc + 1) * 128], in_=HROW[:, fc * 128:(fc + 1) * 128],
                                                       identity=c_ident),
                     reads=["hrow", "ident"], writes=["psb"], inc=(fc == 3))
            P.op("act", lambda e: e.activation(out=HTE, in_=PSB[:, 0:512].rearrange("p (c r) -> p c r", c=4), func=AF.Copy),
                 reads=["psb"], writes=["hte"])

        def exp_C(ex, units):
            el = ex % 2
            for ds in range(4):
                b = bankA()
                ps = psf[b]
                for fc in range(4):
                    wv, wk = units[4 + fc // 2]
                    P.op("pe", lambda e, fc=fc, ds=ds, ps=ps, wv=wv: e.matmul(
                        ps, lhsT=HTE[:, fc, :], rhs=wv[:, fc % 2, ds * 512:(ds + 1) * 512], start=(fc == 0), stop=(fc == 3)),
                        reads=["hte", wk], writes=["psf%d" % b], inc=(fc == 3))
                if ds % 2 == 0:
                    P.op("act", lambda e, ps=ps, ds=ds: e.activation(out=YG[:, el, ds * 512:(ds + 1) * 512], in_=ps, func=AF.Copy),
                         reads=["psf%d" % b], writes=["yg%d" % el])
                else:
                    P.op("dve", lambda e, ps=ps, ds=ds: e.tensor_copy(out=YG[:, el, ds * 512:(ds + 1) * 512], in_=ps),
                         reads=["psf%d" % b], writes=["yg%d" % el])

        def scatter_pair():
            for c in range(8):
                for ds in range(4):
                    b = 4 + sc_i[0] % 2
                    sc_i[0] += 1
                    ps = psf[b]
                    for el in range(2):
                        P.op("pe", lambda e, ps=ps, el=el, c=c, ds=ds: e.matmul(
                            ps, lhsT=SELT[:, el, c * 128:(c + 1) * 128], rhs=YG[:, el, ds * 512:(ds + 1) * 512],
                            start=(el == 0), stop=(el == 1)),
                            reads=["selt%d" % el, "yg%d" % el], writes=["psf%d" % b], inc=(el == 1))
                    hs = H[:, c, ds * 512:(ds + 1) * 512]
                    P.op("dve", lambda e, hs=hs, ps=ps: e.tensor_tensor(out=hs, in0=hs, in1=ps, op=ALU.add),
                         reads=["psf%d" % b, "H%d" % c], writes=["H%d" % c])

        pump()
        prep_sel(0)
        prep_gather(0, 0)
        prep_gather(0, 1)
        prep_selt(0)
        for ex in range(NEXP):
            if ex + 1 < NEXP:
                prep_sel(ex + 1)
            exp_A(ex, units_of(ex))
            ld_state["consumed"] = 6 * ex + 3
            pump()
            if ex + 1 < NEXP:
                prep_gather(ex + 1, 0)
            exp_B(ex)
            exp_C(ex, units_of(ex))
            ld_state["consumed"] = 6 * ex + 5
            pump()
            if ex + 1 < NEXP:
                prep_gather(ex + 1, 1)
            if ex % 2 == 1:
                scatter_pair()
            if ex + 1 < NEXP:
                prep_selt(ex + 1)

        P.barrier()
        P.dma("sp", c_g, g3.partition_broadcast(128), writes=["g"])
        OST = [A32(100 + 8 * r, 2048) for r in range(2)]
        JUNK3 = A16(116, 2048)
        for tc in range(8):
            s = tc % 2
            st = c_st[:, 40 + 4 * s:44 + 4 * s]
            P.op("act", lambda e, tc=tc, st=st: e.activation(out=JUNK3, in_=H[:, tc, :], func=AF.Square, accum_out=st[:, 0:1]),
                 reads=["H%d" % tc], writes=["junk3", "f_ss%d" % s])
            P.op("act", lambda e, st=st: e.activation(out=st[:, 1:2], in_=st[:, 0:1], func=AF.Sqrt, bias=c_eps, scale=1.0 / D),
                 reads=["f_ss%d" % s, "eps"], writes=["f_rms%d" % s])
            P.op("dve", lambda e, st=st: e.reciprocal(out=st[:, 2:3], in_=st[:, 1:2]), reads=["f_rms%d" % s], writes=["f_rstd%d" % s])
            P.op("dve", lambda e, tc=tc, st=st, s=s: e.scalar_tensor_tensor(
                out=OST[s], in0=H[:, tc, :], scalar=st[:, 2:3], in1=c_g, op0=ALU.mult, op1=ALU.mult),
                reads=["H%d" % tc, "f_rstd%d" % s, "g"], writes=["ost%d" % s])
            P.dma("sp", out_d[tc * 128:(tc + 1) * 128, :], OST[s], reads=["ost%d" % s], writes=["outd"])

        P.barrier(force=True)

        with nc.Block() as block:
            @block.tensor
            def _(e):
                for f in P.ops["pe"]:
                    f()

            @block.scalar
            def _(e):
                for f in P.ops["act"]:
                    f()

            @block.vector
            def _(e):
                for f in P.ops["dve"]:
                    f()

            @block.gpsimd
            def _(e):
                for f in P.ops["pool"]:
                    f()

            @block.sync
            def _(e):
                for f in P.ops["sp"]:
                    f()
    return nc


DBG_STAGE = int(os.environ.get("KDBG", "0"))
KPART = os.environ.get("KPART", "")


def _core_layout(core):
    b = core // 2
    own = A_BLOCKS if core % 2 == 0 else B_BLOCKS
    oth = B_BLOCKS if core % 2 == 0 else A_BLOCKS
    return b, own, oth


def make_in_maps(inputs):
    x = np.ascontiguousarray(inputs["x"], dtype=np.float32)
    f = lambda a: np.ascontiguousarray(np.asarray(a), dtype=np.float32)
    w_rt = np.concatenate([f(inputs["w_coarse"][0]), f(inputs["w_fine"][0]).transpose(1, 0, 2).reshape(D, 32)], axis=1)
    b_rt = np.concatenate([f(inputs["b_coarse"][0]).reshape(4), f(inputs["b_fine"][0]).reshape(32)]).reshape(1, 36)
    lamv = np.concatenate([f(inputs["lambda_q1"][0]), f(inputs["lambda_k1"][0]),
                           f(inputs["lambda_q2"][0]), f(inputs["lambda_k2"][0])]).reshape(1, 256)
    shared = {
        "g1": f(inputs["norm1_g"]).reshape(1, D), "g2": f(inputs["norm2_g"]).reshape(1, D),
        "g3": f(inputs["final_norm_g"]).reshape(1, D),
        "w_in": f(inputs["w_in"][0]), "lamv": lamv, "subg": f(inputs["subln_g"]).reshape(1, 128),
        "pool_w": f(inputs["pool_w"][0]), "pool_scale": np.ascontiguousarray(f(inputs["pool_scale"]).reshape(8, 128).T),
        "w_out": f(inputs["w_out"][0]), "w_rt": np.ascontiguousarray(w_rt), "b_rt": b_rt,
        "w_gate": f(inputs["w_gate"][0]), "w_up": f(inputs["w_up"][0]), "w_down": f(inputs["w_down"][0]),
        "ident": np.eye(128, dtype=np.float32),
        "iota": np.ascontiguousarray(np.broadcast_to(np.arange(128, dtype=np.float32)[None, :], (128, 128))),
        "ustr": np.triu(np.ones((128, 128), dtype=np.float32), 1),
        "trimask": np.triu(np.ones((128, 128), dtype=np.float32)),
    }
    slopes = np.exp2(-8.0 * np.arange(1, 9, dtype=np.float64) / 8.0)
    maps = []
    for core in range(NCORES):
        b, own, oth = _core_layout(core)
        slots = own + oth
        xa = np.zeros((17 * 128, D), dtype=np.float32)
        for si, blk in enumerate(slots):
            xa[si * 128:(si + 1) * 128] = x[b, blk * 128:(blk + 1) * 128]
        for i, blk in enumerate(own):
            if blk > 0:
                xa[16 * 128 + i * 16:16 * 128 + (i + 1) * 16] = x[b, blk * 128 - 16:blk * 128]
        xo = np.ascontiguousarray(xa[0:1024])
        ab = np.zeros((128, 8, 16, 8), dtype=np.float32)
        kk = np.arange(128, dtype=np.float64)
        for i in range(8):
            ref = own[i] * 128 + 64
            for j in range(16):
                if slots[j] > own[i]:
                    ab[:, i, j, :] = NEG
                else:
                    ab[:, i, j, :] = (slopes[None, :] * (slots[j] * 128 + kk[:, None] - ref)).astype(np.float32)
        pf = np.ones((128, 4, 8, 16), dtype=np.float32)
        for gi, w in enumerate((2, 4, 8, 16)):
            for i, blk in enumerate(own):
                pos = blk * 128 + np.arange(16)
                cnt = np.minimum(pos + 1, w).astype(np.float32)
                pf[:, gi, i, :] = (w / cnt)[None, :]
        m = dict(shared)
        m["xall"] = xa
        m["xown"] = xo
        m["abias"] = ab.reshape(128, 1024)
        m["poolfix"] = pf.reshape(128, 512)
        maps.append(m)
    return maps


def kernel(**inputs):
    nc = build_nc()
    in_maps = make_in_maps(inputs)
    res = run_bass_kernel_spmd(nc, in_maps, core_ids=list(range(NCORES)))
    out = np.zeros((4, S, D), dtype=np.float32)
    for core in range(NCORES):
        b, own, _ = _core_layout(core)
        o = res.results[core]["out"]
        for i, blk in enumerate(own):
            out[b, blk * 128:(blk + 1) * 128] = o[i * 128:(i + 1) * 128]
    return out
```

```python
import math
import os
from contextlib import ExitStack

import numpy as np
import concourse.bass as bass
import concourse.mybir as mybir
from concourse.bass_utils import run_bass_kernel_spmd

F32 = mybir.dt.float32
BF16 = mybir.dt.bfloat16
AF = mybir.ActivationFunctionType
ALU = mybir.AluOpType
AX = mybir.AxisListType

D = 2048
DC = 16
S = 2048
NCORES = 8
NEXP = 32
DFF = 512
A_BLOCKS = [0, 3, 4, 7, 8, 11, 12, 15]
B_BLOCKS = [1, 2, 5, 6, 9, 10, 13, 14]
NEG = -30000.0
EPS = 1e-6
N_DMA_SEMS = 24


class Sched:
    ENGS = ("pe", "act", "dve", "pool", "sp")

    def __init__(self, nc, esems, dsems):
        self.nc = nc
        self.eng = {"pe": nc.tensor, "act": nc.scalar, "dve": nc.vector,
                    "pool": nc.gpsimd, "sp": nc.sync}
        self.esem = esems
        self.dsem = dsems
        self.ops = {e: [] for e in self.ENGS}
        self.cnt = {e: 0 for e in self.ENGS}
        self.dcnt = [0] * len(dsems)
        self.waited = {e: {} for e in self.ENGS}
        self.lastw = {}
        self.readers = {}
        self.dma_rr = 0
        self.dma_rr_sw = 0
        self.pending = {e: [] for e in self.ENGS}
        self.enabled = True

    def _sem(self, key):
        return self.esem[key] if isinstance(key, str) else self.dsem[key]

    def _collect(self, eng, reads, writes, is_dma):
        need = {}

        def add(ev):
            if ev is None:
                return
            k, v = ev
            if need.get(k, 0) < v:
                need[k] = v
        for b in reads:
            add(self.lastw.get(b))
        for b in writes:
            add(self.lastw.get(b))
            for ev in self.readers.get(b, ()):
                add(ev)
        out = []
        for k, v in need.items():
            if k == "pe" and eng == "pe" and not is_dma:
                continue
            if self.waited[eng].get(k, 0) >= v:
                continue
            self.waited[eng][k] = v
            out.append((k, v))
        return out

    def _record(self, ev, reads, writes):
        for b in writes:
            self.lastw[b] = ev
            self.readers[b] = []
        for b in reads:
            self.readers.setdefault(b, []).append(ev)

    def op(self, eng, fn, reads=(), writes=(), inc=True):
        if not self.enabled:
            return None
        waits = self._collect(eng, reads, writes, False)
        ev = (eng, self.cnt[eng] + 1)
        if inc:
            self.cnt[eng] += 1
        self._record(ev, reads, writes)
        e = self.eng[eng]
        sem = self.esem[eng]
        wl = [(self._sem(k), v) for k, v in waits]

        def emit():
            for s_, v_ in wl:
                e.wait_ge(s_, v_)
            ins = fn(e)
            if inc:
                ins.then_inc(sem, 1)
        self.ops[eng].append(emit)
        return ev

    def dma(self, q, out, in_, reads=(), writes=()):
        if not self.enabled:
            return None
        waits = self._collect(q, reads, writes, True)
        half = len(self.dsem) // 2
        if q == "pool":
            j = half + self.dma_rr_sw
            self.dma_rr_sw = (self.dma_rr_sw + 1) % half
        else:
            j = self.dma_rr
            self.dma_rr = (self.dma_rr + 1) % half
        self.dcnt[j] += 16
        ev = (j, self.dcnt[j])
        self._record(ev, reads, writes)
        e = self.eng[q]
        sem = self.dsem[j]
        wl = [(self._sem(k), v) for k, v in waits]

        def emit():
            for s_, v_ in wl:
                e.wait_ge(s_, v_)
            e.dma_start(out=out, in_=in_).then_inc(sem, 16)
        self.ops[q].append(emit)
        return ev

    def wait_all(self, eng, events):
        wl = []
        for k, v in events:
            if self.waited[eng].get(k, 0) < v:
                self.waited[eng][k] = v
                wl.append((self._sem(k), v))
        e = self.eng[eng]

        def emit():
            for s_, v_ in wl:
                e.wait_ge(s_, v_)
        self.ops[eng].append(emit)

    def barrier(self, force=False):
        if not self.enabled and not force:
            return
        evs = [(e, self.cnt[e]) for e in self.ENGS if self.cnt[e] > 0]
        evs += [(j, self.dcnt[j]) for j in range(len(self.dsem)) if self.dcnt[j] > 0]
        for e in self.ENGS:
            self.wait_all(e, evs)
        self.lastw.clear()
        self.readers.clear()


def build_nc(dbg=None):
    nc = bass.Bass("TRN2", target_bir_lowering=False)

    def din(name, shape, dt=F32):
        return nc.dram_tensor(name, list(shape), dt, kind="ExternalInput").ap()

    xall = din("xall", [17 * 128, D])
    xown = din("xown", [1024, D])
    g1 = din("g1", [1, D])
    g2 = din("g2", [1, D])
    g3 = din("g3", [1, D])
    w_in = din("w_in", [D, 4096])
    lamv = din("lamv", [1, 256])
    subg = din("subg", [1, 128])
    pool_w = din("pool_w", [4, 256, 256])
    pool_scale = din("pool_scale", [128, 8])
    w_out = din("w_out", [D, D])
    w_rt = din("w_rt", [D, 36])
    b_rt = din("b_rt", [1, 36])
    w_gate = din("w_gate", [NEXP, D, DFF])
    w_up = din("w_up", [NEXP, D, DFF])
    w_down = din("w_down", [NEXP, DFF, D])
    abias = din("abias", [128, 8 * 16 * 8])
    trimask = din("trimask", [128, 128])
    poolfix = din("poolfix", [128, 4 * 8 * 16])
    ident_in = din("ident", [128, 128])
    iota_in = din("iota", [128, 128])
    ustr_in = din("ustr", [128, 128])
    out_d = nc.dram_tensor("out", [1024, D], F32, kind="ExternalOutput").ap()
    dbg_d = None
    if dbg is not None:
        dbg_d = nc.dram_tensor("dbg", list(dbg), F32, kind="ExternalOutput").ap()

    lam_init = 0.8 - 0.6 * math.exp(-0.3 * 0)

    with ExitStack() as es:
        esems = {e: es.enter_context(nc.semaphore("s_" + e)) for e in Sched.ENGS}
        dsems = [es.enter_context(nc.semaphore("d%d" % i)) for i in range(N_DMA_SEMS)]
        P = Sched(nc, esems, dsems)

        ARENA_K = 184
        arena = nc.alloc_sbuf_tensor("arena", [128, ARENA_K * 512], BF16)
        cst = nc.alloc_sbuf_tensor("cst", [128, 5120], F32)
        pall = nc.alloc_psum_tensor("pall", [128, 4096], F32)
        psf = [pall[:, i * 512:(i + 1) * 512] for i in range(7)]
        psb = pall[:, 7 * 512:8 * 512].bitcast(BF16)

        def A16(off_k, n):
            o = int(off_k * 512)
            return arena[:, o:o + n]

        def A32(off_k, n):
            o = int(off_k * 512)
            return arena[:, o:o + 2 * n].bitcast(F32)

        C = cst[:]
        c_ident = C[:, 0:64].bitcast(BF16)
        c_tri = C[:, 64:128].bitcast(BF16)
        c_eps = C[:, 128:129]
        c_lam = C[:, 129:130]
        c_nlam = C[:, 130:131]
        c_tmp = C[:, 132:140]
        c_lamv = C[:, 140:396]
        c_subg = C[:, 396:524]
        c_pscale = C[:, 524:532]
        c_brt = C[:, 532:568]
        c_abias = C[:, 568:1592]
        c_pfix = C[:, 1592:2104]
        c_ss = C[:, 2104:2136]
        c_g = C[:, 2136:4184]
        c_ones = C[:, 4184:4185]
        c_rt = C[:, 4200:5120]

        tmpi = A32(115.5, 128)
        P.dma("sp", tmpi, ident_in, writes=["tmpi"])
        P.op("dve", lambda e: e.tensor_copy(out=c_ident, in_=tmpi), reads=["tmpi"], writes=["ident"])
        P.op("dve", lambda e: e.memset(c_eps, EPS), writes=["eps"])
        P.op("dve", lambda e: e.memset(c_ones, 1.0), writes=["ones"])
        P.dma("sp", c_g, g1.partition_broadcast(128), writes=["g"])
        QP = A16(0, 8 * 2 * 1024).rearrange("p (h a t) -> p h a t", h=8, a=2)
        KT = A16(32, 8 * 2048).rearrange("p (c t) -> p c t", c=8)
        V = A16(64, 16 * 8 * 129).rearrange("p (c h v) -> p c h v", c=16, h=8)
        UT = A16(97, 8 * 1152).rearrange("p (c t) -> p c t", c=8)
        XNT = A16(116, 16 * 1152).rearrange("p (c t) -> p c t", c=16)
        XS = [A32(152, 2048), A32(82, 2048)]
        XN = [A16(160, 2048), A16(164, 2048)]
        WS = [A16(168, 16 * 256).rearrange("p (c f) -> p c f", c=16),
              A16(176, 16 * 256).rearrange("p (c f) -> p c f", c=16)]
        NWS = 2

        PSB = psb
        PSB2 = pall[:, 6 * 512:7 * 512].bitcast(BF16)
        mm_rr = [0]

        def mm_bank():
            b = mm_rr[0] % 4
            mm_rr[0] += 1
            return b

        def norm_chunk(src_ap, ci, dst_tok0, ntok_dst=None):
            s = ci % 2
            xs, xn = XS[s], XN[s]
            P.dma("sp", xs, src_ap, writes=["xs%d" % s])
            P.op("act", lambda e: e.activation(out=xn, in_=xs, func=AF.Square, accum_out=c_ss[:, s:s + 1]),
                 reads=["xs%d" % s], writes=["xn%d" % s, "ss%d" % s])
            P.op("act", lambda e: e.activation(out=c_ss[:, 2 + s:3 + s], in_=c_ss[:, s:s + 1], func=AF.Sqrt,
                                               bias=c_eps, scale=1.0 / D),
                 reads=["ss%d" % s, "eps"], writes=["rms%d" % s])
            P.op("dve", lambda e: e.reciprocal(out=c_ss[:, 4 + s:5 + s], in_=c_ss[:, 2 + s:3 + s]),
                 reads=["rms%d" % s], writes=["rstd%d" % s])
            P.op("dve", lambda e: e.scalar_tensor_tensor(out=xn, in0=xs, scalar=c_ss[:, 4 + s:5 + s], in1=c_g,
                                                         op0=ALU.mult, op1=ALU.mult),
                 reads=["xs%d" % s, "rstd%d" % s, "g"], writes=["xn%d" % s])
            for k in range(2):
                pb, pk = (PSB, "psb") if k == 0 else (PSB2, "psf6")
                for j in range(8):
                    dc = k * 8 + j
                    P.op("pe", lambda e, dc=dc, j=j, pb=pb: e.transpose(out=pb[:, j * 128:(j + 1) * 128],
                                                                       in_=xn[:, dc * 128:(dc + 1) * 128], identity=c_ident),
                         reads=["xn%d" % s, "ident"], writes=[pk], inc=(j == 7))
                dst = XNT[:, k * 8:(k + 1) * 8, dst_tok0:dst_tok0 + 128]
                eng = "act" if k == 0 else "dve"
                if eng == "act":
                    P.op("act", lambda e, dst=dst, pb=pb: e.activation(out=dst, in_=pb.rearrange("p (c t) -> p c t", c=8), func=AF.Copy),
                         reads=[pk], writes=["xnt"])
                else:
                    P.op("dve", lambda e, dst=dst, pb=pb: e.tensor_copy(out=dst, in_=pb.rearrange("p (c t) -> p c t", c=8)),
                         reads=[pk], writes=["xnt"])

        wslab_i = [0]

        def load_wslab(col0, ncols=256):
            s = wslab_i[0] % NWS
            wslab_i[0] += 1
            src = w_in[:, col0:col0 + ncols].rearrange("(c p) f -> p c f", p=128)
            P.dma("pool", WS[s][:, :, 0:ncols], src, writes=["ws%d" % s])
            return s

        def proj_featmajor(col0, dst, ntok, tok_src0=0, dst_tok0=0, qp_h0=None):
            s = load_wslab(col0)
            for oc in range(2):
                t0 = 0
                while t0 < ntok:
                    n = min(512, ntok - t0)
                    b = mm_bank()
                    ps = psf[b][:, 0:n]
                    for dc in range(DC):
                        P.op("pe", lambda e, dc=dc, oc=oc, t0=t0, n=n, ps=ps: e.matmul(
                            ps, lhsT=WS[s][:, dc, oc * 128:(oc + 1) * 128],
                            rhs=XNT[:, dc, tok_src0 + t0:tok_src0 + t0 + n], start=(dc == 0), stop=(dc == DC - 1)),
                            reads=["ws%d" % s, "xnt"], writes=["psf%d" % b], inc=(dc == DC - 1))
                    if qp_h0 is not None:
                        hq = qp_h0 + oc
                        P.op("act", lambda e, hq=hq, t0=t0, n=n, ps=ps: e.activation(
                            out=QP[0:64, hq, 0, t0:t0 + n], in_=ps[0:64, :], func=AF.Copy),
                            reads=["psf%d" % b], writes=["projout"])
                        P.op("dve", lambda e, hq=hq, t0=t0, n=n, ps=ps: e.tensor_copy(
                            out=QP[64:128, hq, 1, t0:t0 + n], in_=ps[64:128, :]),
                            reads=["psf%d" % b], writes=["projout"])
                        t0 += n
                        continue
                    d_ap = dst[:, oc, dst_tok0 + t0:dst_tok0 + t0 + n]
                    if (mm_rr[0] % 2) == 0:
                        P.op("act", lambda e, d_ap=d_ap, ps=ps: e.activation(out=d_ap, in_=ps, func=AF.Copy),
                             reads=["psf%d" % b], writes=["projout"])
                    else:
                        P.op("dve", lambda e, d_ap=d_ap, ps=ps: e.tensor_copy(out=d_ap, in_=ps),
                             reads=["psf%d" % b], writes=["projout"])
                    t0 += n

        def proj_v(col0, hh, chunk_src0, nchunks, chunk_dst0):
            s = load_wslab(col0)
            for ci in range(nchunks):
                b = mm_bank()
                ps = psf[b][:, 0:256]
                for dc in range(DC):
                    P.op("pe", lambda e, dc=dc, ci=ci, ps=ps: e.matmul(
                        ps, lhsT=XNT[:, dc, (chunk_src0 + ci) * 128:(chunk_src0 + ci + 1) * 128],
                        rhs=WS[s][:, dc, 0:256], start=(dc == 0), stop=(dc == DC - 1)),
                        reads=["ws%d" % s, "xnt"], writes=["psf%d" % b], inc=(dc == DC - 1))
                d_ap = V[:, chunk_dst0 + ci, 2 * hh:2 * hh + 2, 0:128]
                src = ps.rearrange("p (h v) -> p h v", h=2)
                if ci % 2 == 0:
                    P.op("act", lambda e, d_ap=d_ap, src=src: e.activation(out=d_ap, in_=src, func=AF.Copy),
                         reads=["psf%d" % b], writes=["V"])
                else:
                    P.op("dve", lambda e, d_ap=d_ap, src=src: e.tensor_copy(out=d_ap, in_=src),
                         reads=["psf%d" % b], writes=["V"])

        P.op("pool", lambda e: e.memset(V[:, 0:8, :, 128:129], 1.0), writes=["V"])
        P.op("pool", lambda e: e.memset(A16(0, 16384), 0.0), writes=["projout"])
        for ci in range(9):
            src_chunk = ci if ci < 8 else 16
            norm_chunk(xall[src_chunk * 128:(src_chunk + 1) * 128, :], ci, ci * 128)
        tmpt = A32(96.25, 128)
        P.dma("sp", tmpt, trimask, writes=["tmpt"])
        P.op("dve", lambda e: e.tensor_copy(out=c_tri, in_=tmpt), reads=["tmpt"], writes=["tri"])
        P.dma("sp", c_lamv, lamv.partition_broadcast(128), writes=["lamv"])
        P.dma("sp", c_subg, subg.partition_broadcast(128), writes=["subg"])
        P.dma("sp", c_pscale, pool_scale, writes=["pscale"])
        P.dma("sp", c_brt, b_rt.partition_broadcast(128), writes=["brt"])
        P.dma("sp", c_abias, abias, writes=["abias"])
        P.dma("sp", c_pfix, poolfix, writes=["pfix"])
        P.op("dve", lambda e: e.tensor_tensor(out=c_lamv[:, 0:64], in0=c_lamv[:, 0:64], in1=c_lamv[:, 64:128], op=ALU.mult),
             reads=["lamv"], writes=["lamv"])
        P.op("dve", lambda e: e.tensor_tensor(out=c_lamv[:, 128:192], in0=c_lamv[:, 128:192], in1=c_lamv[:, 192:256], op=ALU.mult),
             reads=["lamv"], writes=["lamv"])
        P.op("dve", lambda e: e.tensor_reduce(out=c_tmp[:, 0:1], in_=c_lamv[:, 0:64], axis=AX.X, op=ALU.add),
             reads=["lamv"], writes=["tmp0"])
        P.op("dve", lambda e: e.tensor_reduce(out=c_tmp[:, 1:2], in_=c_lamv[:, 128:192], axis=AX.X, op=ALU.add),
             reads=["lamv"], writes=["tmp1"])
        P.op("act", lambda e: e.activation(out=c_tmp[:, 2:4], in_=c_tmp[:, 0:2], func=AF.Exp),
             reads=["tmp0", "tmp1"], writes=["tmp2"])
        P.op("dve", lambda e: e.tensor_tensor(out=c_lam, in0=c_tmp[:, 2:3], in1=c_tmp[:, 3:4], op=ALU.subtract),
             reads=["tmp2"], writes=["lam"])
        P.op("dve", lambda e: e.tensor_scalar(out=c_lam, in0=c_lam, scalar1=lam_init, scalar2=None, op0=ALU.add),
             reads=["lam"], writes=["lam"])
        P.op("dve", lambda e: e.tensor_scalar(out=c_nlam, in0=c_lam, scalar1=-1.0, scalar2=None, op0=ALU.mult),
             reads=["lam"], writes=["nlam"])
        P.op("dve", lambda e: e.tensor_scalar(out=c_subg, in0=c_subg, scalar1=(1.0 - lam_init), scalar2=None, op0=ALU.mult),
             reads=["subg"], writes=["subg"])
        for oc2 in range(4):
            proj_featmajor(oc2 * 256, None, 1024, qp_h0=2 * oc2)
        for oc2 in range(4):
            proj_featmajor(1024 + oc2 * 256, KT[:, 2 * oc2:2 * oc2 + 2, :], 1024)
        for hh in range(4):
            proj_v(2048 + hh * 256, hh, 0, 8, 0)
        UTv = UT.rearrange("p c (b t) -> p c b t", b=8)
        for oc2 in range(4):
            s = load_wslab(3072 + oc2 * 256)
            for oc in range(2):
                for half in range(3):
                    t0 = half * 512
                    n = 512 if half < 2 else 128
                    b = mm_bank()
                    ps = psf[b][:, 0:n]
                    for dc in range(DC):
                        P.op("pe", lambda e, dc=dc, oc=oc, t0=t0, n=n, ps=ps, s=s: e.matmul(
                            ps, lhsT=WS[s][:, dc, oc * 128:(oc + 1) * 128], rhs=XNT[:, dc, t0:t0 + n],
                            start=(dc == 0), stop=(dc == DC - 1)),
                            reads=["ws%d" % s, "xnt"], writes=["psf%d" % b], inc=(dc == DC - 1))
                    if half < 2:
                        d_ap = UTv[:, 2 * oc2 + oc, half * 4:half * 4 + 4, 16:144]
                        src = ps.rearrange("p (b t) -> p b t", b=4)
                    else:
                        d_ap = UTv[:, 2 * oc2 + oc, :, 0:16]
                        src = ps.rearrange("p (b t) -> p b t", b=8)
                    P.op("dve", lambda e, d_ap=d_ap, src=src: e.tensor_copy(out=d_ap, in_=src),
                         reads=["psf%d" % b], writes=["UT"])
        for ci in range(8):
            norm_chunk(xall[(8 + ci) * 128:(9 + ci) * 128, :], ci, ci * 128)
        P.op("pool", lambda e: e.memset(V[:, 8:16, :, 128:129], 1.0), writes=["V", "xs1"])
        for oc2 in range(4):
            proj_featmajor(1024 + oc2 * 256, KT[:, 2 * oc2:2 * oc2 + 2, :], 1024, dst_tok0=1024)
        for hh in range(4):
            proj_v(2048 + hh * 256, hh, 0, 8, 8)


        P.barrier()
        MT = A16(116, 16 * 1024).rearrange("p (c t) -> p c t", c=16)
        ET = [A16(148 + 0.5 * r, 256) for r in range(4)]
        O1 = [A32(150 + 0.5 * r, 128) for r in range(2)]
        O2 = [A32(151 + 0.5 * r, 128) for r in range(2)]
        AO = A16(152, 2 * 1024).rearrange("p (b f) -> p b f", b=2)
        TB = [A32(156 + 4.5 * r, 1152).rearrange("p (b t) -> p b t", b=8) for r in range(3)]
        PT = [A16(170 + 4 * r, 2048).rearrange("p (c t) -> p c t", c=2) for r in range(2)]
        PW = A16(178, 4 * 2 * 256).rearrange("p (g c e) -> p g c e", g=4, c=2)
        c_st = C[:, 4200:4300]

        P.dma("pool", PW, pool_w.rearrange("g (c p) e -> p g c e", p=128), writes=["PW"])

        if KPART == 'attn':
            P.enabled = False
        UTv2 = UT.rearrange("p c (b t) -> p c b t", b=8)
        for c in range(8):
            g = c // 2
            w = 2 << g
            u = UTv2[:, c]
            cur = u
            sh = 1
            k = 0
            while sh < w:
                dst = TB[k % 2]
                P.op("dve", lambda e, dst=dst, cur=cur, sh=sh: e.tensor_tensor(
                    out=dst[:, :, sh:144], in0=cur[:, :, sh:144], in1=cur[:, :, 0:144 - sh], op=ALU.add),
                    reads=["UT", "TB%d" % ((k + 1) % 2)], writes=["TB%d" % (k % 2)])
                if sh > 1 or True:
                    pass
                cur = dst
                sh *= 2
                k += 1
            last = (k - 1) % 2
            t2 = TB[2]
            P.op("dve", lambda e, cur=cur, w=w, t2=t2: e.tensor_scalar(
                out=t2[:, :, 0:128], in0=cur[:, :, 16:144], scalar1=1.0 / w, scalar2=None, op0=ALU.mult),
                reads=["TB%d" % last], writes=["TB2"])
            pf = c_pfix.rearrange("p (g b t) -> p g b t", g=4, b=8)[:, g]
            P.op("dve", lambda e, t2=t2, pf=pf: e.tensor_tensor(
                out=t2[:, :, 0:16], in0=t2[:, :, 0:16], in1=pf, op=ALU.mult),
                reads=["TB2", "pfix"], writes=["TB2"])
            ptd = PT[g % 2][:, c % 2, :].rearrange("p (b t) -> p b t", b=8)
            P.op("dve", lambda e, t2=t2, u=u, ptd=ptd: e.tensor_tensor(
                out=ptd, in0=t2[:, :, 0:128], in1=u[:, :, 16:144], op=ALU.subtract),
                reads=["TB2", "UT"], writes=["PT%d_%d" % (g % 2, c % 2)])
            if c % 2 == 1:
                for ec in range(2):
                    for th in range(2):
                        b = 6
                        ps = psf[b][:, 0:512]
                        for cc in range(2):
                            P.op("pe", lambda e, g=g, cc=cc, ec=ec, th=th, ps=ps: e.matmul(
                                ps, lhsT=PW[:, g, cc, ec * 128:(ec + 1) * 128],
                                rhs=PT[g % 2][:, cc, th * 512:(th + 1) * 512], start=(cc == 0), stop=(cc == 1)),
                                reads=["PW", "PT%d_0" % (g % 2), "PT%d_1" % (g % 2)], writes=["psf%d" % b], inc=(cc == 1))
                        P.op("act", lambda e, g=g, ec=ec, th=th, ps=ps: e.activation(
                            out=MT[:, 8 + 2 * g + ec, th * 512:(th + 1) * 512], in_=ps, func=AF.Copy,
                            scale=c_pscale[:, 2 * g + ec:2 * g + ec + 1]),
                            reads=["psf%d" % b, "pscale"], writes=["MT"])

        P.enabled = (KPART != 'pool')
        ABv = c_abias.rearrange("p (i j h) -> p i j h", i=8, j=16)
        SB = [psf[4][:, 0:256], psf[5][:, 0:256], psf[6][:, 0:256]]
        ACC = [(psf[0], psf[1]), (psf[2], psf[3])]
        items = []
        for i in range(8):
            for h in range(8):
                js_list = list(range(i + 1)) + [8 + j for j in range(i + 1)]
                for n_, js in enumerate(js_list):
                    items.append((i, h, js, n_ == 0, n_ == len(js_list) - 1))

        def emit_S(n):
            i, h, js, first, last = items[n]
            sb = SB[n % 3]
            P.op("pe", lambda e: e.matmul(sb.rearrange("p (a q) -> p a q", a=2), lhsT=KT[:, h, js * 128:(js + 1) * 128],
                                          rhs=QP[:, h, :, i * 128:(i + 1) * 128], start=True, stop=True),
                 reads=["KT", "projout"], writes=["psf%d" % (4 + n % 3)], inc=True)

        gi_ = [0]

        def emit_rest(n):
            i, h, js, first, last = items[n]
            sb = SB[n % 3]
            et = ET[n % 4]
            bias = ABv[:, i, js, h:h + 1]
            P.op("act", lambda e: e.activation(out=et, in_=sb, func=AF.Exp, bias=bias, scale=0.125),
                 reads=["psf%d" % (4 + n % 3), "abias"], writes=["E%d" % (n % 4)])
            if js == i:
                et3 = et.rearrange("p (a q) -> p a q", a=2)
                tri3 = c_tri.unsqueeze(1).broadcast_to([128, 2, 128])
                P.op("dve", lambda e: e.tensor_tensor(out=et3, in0=et3, in1=tri3, op=ALU.mult),
                     reads=["E%d" % (n % 4), "tri"], writes=["E%d" % (n % 4)])
            gi = gi_[0]
            a1, a2 = ACC[gi % 2]
            P.op("pe", lambda e: e.matmul(a1[:, 0:129], lhsT=et[:, 0:128], rhs=V[:, js, h, :], start=first, stop=last),
                 reads=["E%d" % (n % 4), "V"], writes=["acc%d" % (gi % 2)], inc=False)
            P.op("pe", lambda e: e.matmul(a2[:, 0:129], lhsT=et[:, 128:256], rhs=V[:, js, h, :], start=first, stop=last),
                 reads=["E%d" % (n % 4), "V"], writes=["acc%d" % (gi % 2)], inc=True)
            if last:
                fin_q.extend([(gi, f_) for f_ in make_finalize(i, h, gi % 2, a1, a2)])
                gi_[0] += 1

        fin_q = []

        def make_finalize(i, h, r, a1, a2):
            st = c_st[:, 10 * r:10 * r + 10]
            ak = "acc%d" % r
            o1, o2 = O1[r], O2[r]

            def d1():
                P.op("dve", lambda e: e.reciprocal(out=st[:, 0:1], in_=a1[:, 128:129]), reads=[ak], writes=["st%d" % r])
                P.op("dve", lambda e: e.reciprocal(out=st[:, 1:2], in_=a2[:, 128:129]), reads=[ak], writes=["st%d" % r])
                P.op("dve", lambda e: e.tensor_tensor(out=st[:, 2:3], in0=st[:, 1:2], in1=c_nlam, op=ALU.mult),
                     reads=["st%d" % r, "nlam"], writes=["st%d" % r])

            def a1s():
                P.op("dve", lambda e: e.tensor_scalar(out=o1, in0=a1[:, 0:128], scalar1=st[:, 0:1], scalar2=None, op0=ALU.mult),
                     reads=[ak, "st%d" % r], writes=["o1_%d" % r])

            def d2():
                P.op("dve", lambda e: e.scalar_tensor_tensor(out=o2, in0=a2[:, 0:128], scalar=st[:, 2:3], in1=o1,
                                                             op0=ALU.mult, op1=ALU.add),
                     reads=[ak, "st%d" % r, "o1_%d" % r], writes=["o2_%d" % r])

            def a23():
                P.op("dve", lambda e: e.tensor_tensor(out=o1, in0=o2, in1=o2, op=ALU.mult),
                     reads=["o2_%d" % r], writes=["o1_%d" % r])
                P.op("dve", lambda e: e.tensor_reduce(out=st[:, 3:4], in_=o1, axis=AX.X, op=ALU.add),
                     reads=["o1_%d" % r], writes=["sq%d" % r])
                P.op("act", lambda e: e.activation(out=st[:, 4:5], in_=st[:, 3:4], func=AF.Ln, bias=c_eps, scale=1.0 / 128),
                     reads=["sq%d" % r, "eps"], writes=["rm%d" % r])
                P.op("act", lambda e: e.activation(out=st[:, 5:6], in_=st[:, 4:5], func=AF.Exp, scale=-0.5),
                     reads=["rm%d" % r], writes=["rs%d" % r])

            def d3():
                P.op("dve", lambda e: e.scalar_tensor_tensor(out=AO[:, i % 2, h * 128:(h + 1) * 128], in0=o2, scalar=st[:, 5:6],
                                                             in1=c_subg, op0=ALU.mult, op1=ALU.mult),
                     reads=["o2_%d" % r, "rs%d" % r, "subg"], writes=["AO%d" % (i % 2)])

            def tr():
                for hh in range(8):
                    P.op("pe", lambda e, hh=hh: e.transpose(out=PSB[:, hh * 128:(hh + 1) * 128],
                                                           in_=AO[:, i % 2, hh * 128:(hh + 1) * 128], identity=c_ident),
                         reads=["AO%d" % (i % 2), "ident"], writes=["psb"], inc=(hh == 7))
                P.op("dve", lambda e: e.tensor_copy(out=MT[:, 0:8, i * 128:(i + 1) * 128],
                                                    in_=PSB.rearrange("p (c t) -> p c t", c=8)),
                     reads=["psb"], writes=["MT"])
            stages = [d1, a1s, d2, a23, d3]
            if h == 7:
                stages.append(tr)
            return stages

        NI = len(items)
        emit_S(0)
        if NI > 1:
            emit_S(1)
        for n in range(NI):
            if n + 2 < NI:
                emit_S(n + 2)
            if items[n][3]:
                while fin_q and fin_q[0][0] <= gi_[0] - 2:
                    fin_q.pop(0)[1]()
            emit_rest(n)
            if fin_q:
                fin_q.pop(0)[1]()
        while fin_q:
            fin_q.pop(0)[1]()

        P.enabled = True
        if DBG_STAGE == 2:
            P.barrier()
            stg = A32(156, 1024)
            for c in range(16):
                P.op("dve", lambda e, c=c: e.tensor_copy(out=stg, in_=MT[:, c, :]), reads=[], writes=["stg"])
                P.dma("sp", dbg_d[c * 128:(c + 1) * 128, 0:1024], stg, reads=["stg"], writes=["dbgout"])
            P.barrier()
            P.enabled = False
        P.barrier()
        H = A32(0, 8 * 2048).rearrange("p (c d) -> p c d", c=8)
        HNT = A16(116, 16 * 1024).rearrange("p (c t) -> p c t", c=16)
        HNK = A16(64, 8 * 2048).rearrange("p (c d) -> p c d", c=8)
        WO = [A16(148 + 16 * r, 16 * 512).rearrange("p (c f) -> p c f", c=16) for r in range(2)]
        JUNK2 = A16(108, 2048)
        for tc in range(8):
            P.dma("sp", H[:, tc, :], xown[tc * 128:(tc + 1) * 128, :], writes=["H%d" % tc])
        P.dma("sp", c_g, g2.partition_broadcast(128), writes=["g"])
        for ds in range(4):
            s = ds % 2
            P.dma("pool", WO[s], w_out[:, ds * 512:(ds + 1) * 512].rearrange("(c p) f -> p c f", p=128), writes=["wo%d" % s])
            for tc in range(8):
                b = mm_bank()
                ps = psf[b][:, 0:512]
                for fc in range(16):
                    P.op("pe", lambda e, fc=fc, tc=tc, s=s, ps=ps: e.matmul(
                        ps, lhsT=MT[:, fc, tc * 128:(tc + 1) * 128], rhs=WO[s][:, fc, :], start=(fc == 0), stop=(fc == 15)),
                        reads=["MT", "wo%d" % s], writes=["psf%d" % b], inc=(fc == 15))
                hs = H[:, tc, ds * 512:(ds + 1) * 512]
                P.op("dve", lambda e, hs=hs, ps=ps: e.tensor_tensor(out=hs, in0=hs, in1=ps, op=ALU.add),
                     reads=["psf%d" % b, "H%d" % tc], writes=["H%d" % tc])

        def norm_tok(tc, src, gkey, dst_bf16=None, dst_f32=None, junk=None):
            s = tc % 2
            st = c_st[:, 30 + 4 * s:34 + 4 * s]
            P.op("act", lambda e: e.activation(out=junk, in_=src, func=AF.Square, accum_out=st[:, 0:1]),
                 reads=["H%d" % tc], writes=["junk2", "n_ss%d" % s])
            P.op("act", lambda e: e.activation(out=st[:, 1:2], in_=st[:, 0:1], func=AF.Sqrt, bias=c_eps, scale=1.0 / D),
                 reads=["n_ss%d" % s, "eps"], writes=["n_rms%d" % s])
            P.op("dve", lambda e: e.reciprocal(out=st[:, 2:3], in_=st[:, 1:2]), reads=["n_rms%d" % s], writes=["n_rstd%d" % s])
            return st[:, 2:3], "n_rstd%d" % s

        P.barrier()
        for tc in range(8):
            s = tc % 2
            rstd, rk = norm_tok(tc, H[:, tc, :], "g", junk=JUNK2)
            hn = HNK[:, tc, :]
            P.op("dve", lambda e, hn=hn, tc=tc, rstd=rstd: e.scalar_tensor_tensor(
                out=hn, in0=H[:, tc, :], scalar=rstd, in1=c_g, op0=ALU.mult, op1=ALU.mult),
                reads=["H%d" % tc, rk, "g"], writes=["hnk%d" % tc])
            for k in range(2):
                pb, pk = (PSB, "psb") if k == 0 else (PSB2, "psf6")
                for j in range(8):
                    dc = k * 8 + j
                    P.op("pe", lambda e, dc=dc, j=j, hn=hn, pb=pb: e.transpose(out=pb[:, j * 128:(j + 1) * 128],
                                                                              in_=hn[:, dc * 128:(dc + 1) * 128], identity=c_ident),
                         reads=["hnk%d" % tc, "ident"], writes=[pk], inc=(j == 7))
                dst = HNT[:, k * 8:(k + 1) * 8, tc * 128:(tc + 1) * 128]
                if k == 0:
                    P.op("act", lambda e, dst=dst, pb=pb: e.activation(out=dst, in_=pb.rearrange("p (c t) -> p c t", c=8), func=AF.Copy),
                         reads=[pk], writes=["HNT"])
                else:
                    P.op("dve", lambda e, dst=dst, pb=pb: e.tensor_copy(out=dst, in_=pb.rearrange("p (c t) -> p c t", c=8)),
                         reads=[pk], writes=["HNT"])

        if DBG_STAGE == 3:
            P.barrier()
            for tc in range(8):
                P.dma("sp", dbg_d[tc * 128:(tc + 1) * 128, 0:2048], H[:, tc, :], reads=[], writes=["dbgout"])
            stg = A32(100, 1024)
            for c in range(16):
                P.op("dve", lambda e, c=c: e.tensor_copy(out=stg, in_=HNT[:, c, :]), reads=[], writes=["stg"])
                P.dma("sp", dbg_d[1024 + c * 128:1024 + (c + 1) * 128, 0:1024], stg, reads=["stg"], writes=["dbgout"])
            P.barrier()
            P.enabled = False
        P.barrier()
        NUNIT = 7
        WU_ = [A16(116 + 8 * r, 4096) for r in range(NUNIT)]
        XGB = [A16(96 + 4 * r, 16 * 128).rearrange("p (c r) -> p c r", c=16) for r in range(2)]
        YG = A16(104, 2 * 2048).rearrange("p (e d) -> p e d", e=2)
        SELB = [A16(112 + 2 * r, 8 * 128).rearrange("p (c j) -> p c j", c=8) for r in range(2)]
        SELT = A16(172, 2 * 1024).rearrange("p (e t) -> p e t", e=2)
        HTE = A16(176, 512).rearrange("p (c r) -> p c r", c=4)
        HROW = A16(177, 512)
        SILT = A32(178, 512)
        RS = A32(180, 1024)
        WR = A16(172, 16 * 36).rearrange("p (c n) -> p c n", c=16)
        P.dma("pool", WR, w_rt.rearrange("(c p) n -> p c n", p=128), writes=["wr"])
        RT2 = C[:, 2136:4184]
        c_iota = RT2[:, 0:128]
        c_ustr = RT2[:, 128:192].bitcast(BF16)
        c_onem = RT2[:, 192:256].bitcast(BF16)
        A_bf = RT2[:, 256:384].bitcast(BF16)
        OFF = RT2[:, 384:640]
        POS = RT2[:, 640:896]
        P.dma("sp", c_iota, iota_in, reads=["g"], writes=["iota"])
        tmpu = A32(179, 128)
        P.dma("sp", tmpu, ustr_in, writes=["tmpu"])
        P.op("dve", lambda e: e.tensor_copy(out=c_ustr, in_=tmpu), reads=["tmpu", "g"], writes=["ustr"])
        P.op("dve", lambda e: e.memset(c_onem, 1.0), reads=["g"], writes=["onem"])
        LGP = psf[6][:, 0:288]
        for tc in range(8):
            for dc in range(16):
                P.op("pe", lambda e, tc=tc, dc=dc: e.matmul(LGP[:, tc * 36:(tc + 1) * 36], lhsT=HNT[:, dc, tc * 128:(tc + 1) * 128],
                                                           rhs=WR[:, dc, :], start=(dc == 0), stop=(dc == 15)),
                     reads=["HNT", "wr"], writes=["psf6", "wu0", "wu1", "wu2", "wu3"], inc=(dc == 15 and tc == 7))
        LG = RS[:, 0:288].rearrange("p (t n) -> p t n", t=8)
        FM = RS[:, 288:544]
        M1 = RS[:, 544:800]
        M2 = c_rt[:, 0:256]
        COMB = c_rt[:, 256:512]
        sm = c_rt[:, 512:900]
        cmax = sm[:, 0:8]
        gmask = sm[:, 8:40].rearrange("p (t g) -> p t g", t=8)
        ecx = sm[:, 40:72].rearrange("p (t g) -> p t g", t=8)
        se = sm[:, 72:80]
        pg = sm[:, 80:88]
        pen = sm[:, 88:120].rearrange("p (t g) -> p t g", t=8)
        m1 = sm[:, 120:128]
        m2 = sm[:, 128:136]
        dd = sm[:, 136:144]
        ee = sm[:, 144:152]
        w1 = sm[:, 152:160]
        w2 = sm[:, 160:168]
        BIG = 1.0e9
        rk = ["rt"]

        def R(fn, eng="dve"):
            P.op(eng, fn, reads=rk, writes=rk)
        P.op("dve", lambda e: e.tensor_tensor(out=LG, in0=LGP.rearrange("p (t n) -> p t n", t=8),
                                              in1=c_brt.unsqueeze(1).broadcast_to([128, 8, 36]), op=ALU.add),
             reads=["psf6", "brt"], writes=rk)
        coarse = LG[:, :, 0:4]
        fine = LG[:, :, 4:36].rearrange("p t (g x) -> p t g x", g=4)
        R(lambda e: e.tensor_reduce(out=cmax, in_=coarse, axis=AX.X, op=ALU.max))
        R(lambda e: e.tensor_tensor(out=gmask, in0=coarse, in1=cmax.unsqueeze(2).broadcast_to([128, 8, 4]), op=ALU.is_ge))
        R(lambda e: e.tensor_tensor(out=ecx, in0=coarse, in1=cmax.unsqueeze(2).broadcast_to([128, 8, 4]), op=ALU.subtract))
        R(lambda e: e.activation(out=ecx, in_=ecx, func=AF.Exp), "act")
        R(lambda e: e.tensor_reduce(out=se, in_=ecx, axis=AX.X, op=ALU.add))
        R(lambda e: e.reciprocal(out=pg, in_=se))
        R(lambda e: e.tensor_scalar(out=pen, in0=gmask, scalar1=BIG, scalar2=-BIG, op0=ALU.mult, op1=ALU.add))
        FM4 = FM.rearrange("p (t g x) -> p t g x", t=8, g=4)
        R(lambda e: e.tensor_tensor(out=FM4, in0=fine, in1=pen.unsqueeze(3).broadcast_to([128, 8, 4, 8]), op=ALU.add))
        FM3 = FM.rearrange("p (t n) -> p t n", t=8)
        M13 = M1.rearrange("p (t n) -> p t n", t=8)
        M23 = M2.rearrange("p (t n) -> p t n", t=8)
        CB3 = COMB.rearrange("p (t n) -> p t n", t=8)
        R(lambda e: e.tensor_reduce(out=m1, in_=FM3, axis=AX.X, op=ALU.max))
        R(lambda e: e.tensor_tensor(out=M13, in0=FM3, in1=m1.unsqueeze(2).broadcast_to([128, 8, 32]), op=ALU.is_ge))
        R(lambda e: e.scalar_tensor_tensor(out=FM, in0=M1, scalar=-BIG, in1=FM, op0=ALU.mult, op1=ALU.add))
        R(lambda e: e.tensor_reduce(out=m2, in_=FM3, axis=AX.X, op=ALU.max))
        R(lambda e: e.tensor_tensor(out=M23, in0=FM3, in1=m2.unsqueeze(2).broadcast_to([128, 8, 32]), op=ALU.is_ge))
        R(lambda e: e.tensor_tensor(out=dd, in0=m2, in1=m1, op=ALU.subtract))
        R(lambda e: e.activation(out=ee, in_=dd, func=AF.Exp), "act")
        R(lambda e: e.tensor_scalar(out=dd, in0=ee, scalar1=1.0, scalar2=None, op0=ALU.add))
        R(lambda e: e.reciprocal(out=w1, in_=dd))
        R(lambda e: e.tensor_tensor(out=w2, in0=ee, in1=w1, op=ALU.mult))
        R(lambda e: e.tensor_tensor(out=w1, in0=w1, in1=pg, op=ALU.mult))
        R(lambda e: e.tensor_tensor(out=w2, in0=w2, in1=pg, op=ALU.mult))
        R(lambda e: e.tensor_tensor(out=M13, in0=M13, in1=w1.unsqueeze(2).broadcast_to([128, 8, 32]), op=ALU.mult))
        R(lambda e: e.tensor_tensor(out=M23, in0=M23, in1=w2.unsqueeze(2).broadcast_to([128, 8, 32]), op=ALU.mult))
        R(lambda e: e.tensor_tensor(out=COMB, in0=M1, in1=M2, op=ALU.add))

        if DBG_STAGE == 4:
            P.barrier()
            P.dma("sp", dbg_d[0:128, 0:256], COMB, reads=[], writes=["dbgout"])
            P.dma("sp", dbg_d[128:256, 0:288], RS[:, 0:288], reads=[], writes=["dbgout"])
            P.barrier()
            P.enabled = False
        POS3 = POS.rearrange("p (c e) -> p c e", c=8)
        OFF3 = OFF.rearrange("p (c e) -> p c e", c=8)
        P.op("dve", lambda e: e.tensor_scalar(out=A_bf, in0=COMB, scalar1=0.0, scalar2=None, op0=ALU.is_gt),
             reads=rk + ["g"], writes=["abf"])
        P.op("pe", lambda e: e.matmul(psf[0][:, 0:256], lhsT=c_ustr, rhs=A_bf, start=True, stop=True),
             reads=["abf", "ustr"], writes=["psf0"])
        P.op("pe", lambda e: e.matmul(psf[1][:, 0:256], lhsT=c_onem, rhs=A_bf, start=True, stop=True),
             reads=["abf", "onem"], writes=["psf1"])
        TOT3 = psf[1][:, 0:256].rearrange("p (c e) -> p c e", c=8)
        P.op("dve", lambda e: e.memset(OFF3[:, 0, :], 0.0), reads=["g"], writes=["off"])
        for c in range(1, 8):
            P.op("dve", lambda e, c=c: e.tensor_tensor(out=OFF3[:, c, :], in0=OFF3[:, c - 1, :], in1=TOT3[:, c - 1, :], op=ALU.add),
                 reads=["psf1", "off"], writes=["off"])
        P.op("dve", lambda e: e.tensor_tensor(out=POS, in0=OFF, in1=psf[0][:, 0:256], op=ALU.add),
             reads=["psf0", "off"], writes=["pos"])
        P.op("dve", lambda e: e.tensor_scalar(out=POS, in0=POS, scalar1=1.0, scalar2=None, op0=ALU.add), reads=["pos"], writes=["pos"])
        P.op("dve", lambda e: e.tensor_tensor(out=POS, in0=POS, in1=A_bf, op=ALU.mult), reads=["pos", "abf"], writes=["pos"])
        P.op("dve", lambda e: e.tensor_scalar(out=POS, in0=POS, scalar1=-1.0, scalar2=None, op0=ALU.add), reads=["pos"], writes=["pos"])

        poolA = [0, 1, 6]
        pa_i = [0]

        def bankA():
            b = poolA[pa_i[0] % 3]
            pa_i[0] += 1
            return b
        sc_i = [0]
        un_i = [0]
        iota3 = c_iota.unsqueeze(1).broadcast_to([128, 8, 128])

        def load_unit(src, view, kw):
            k = un_i[0] % NUNIT
            un_i[0] += 1
            dst = WU_[k].rearrange(view, **kw)
            P.dma("pool", dst, src, writes=["wu%d" % k])
            return dst, "wu%d" % k

        def prep_sel(ex):
            sl = SELB[ex % 2]
            P.op("dve", lambda e: e.tensor_tensor(
                out=sl, in0=iota3, in1=POS3[:, :, ex:ex + 1].broadcast_to([128, 8, 128]), op=ALU.is_equal),
                reads=["pos", "iota"], writes=["sel%d" % (ex % 2)])

        def prep_gather(ex, half):
            sl = SELB[ex % 2]
            xg = XGB[ex % 2]
            for q4 in range(2 * half, 2 * half + 2):
                b = bankA()
                ps = psf[b]
                for d4 in range(4):
                    dc = 4 * q4 + d4
                    for c in range(8):
                        P.op("pe", lambda e, ps=ps, d4=d4, dc=dc, c=c: e.matmul(
                            ps[:, d4 * 128:(d4 + 1) * 128], lhsT=HNK[:, c, dc * 128:(dc + 1) * 128],
                            rhs=sl[:, c, :], start=(c == 0), stop=(c == 7)),
                            reads=["hnk%d" % c, "sel%d" % (ex % 2)], writes=["psf%d" % b], inc=(c == 7 and d4 == 3))
                P.op("act", lambda e, ps=ps, q4=q4: e.activation(
                    out=xg[:, 4 * q4:4 * q4 + 4, :], in_=ps.rearrange("p (a r) -> p a r", a=4), func=AF.Copy),
                    reads=["psf%d" % b], writes=["xg%d" % (ex % 2)])

        def prep_selt(ex):
            sl = SELB[ex % 2]
            P.op("dve", lambda e: e.tensor_tensor(
                out=sl, in0=sl, in1=CB3[:, :, ex:ex + 1].broadcast_to([128, 8, 128]), op=ALU.mult),
                reads=["sel%d" % (ex % 2)] + rk, writes=["sel%d" % (ex % 2)])
            for c in range(8):
                P.op("pe", lambda e, c=c: e.transpose(out=PSB[:, c * 128:(c + 1) * 128], in_=sl[:, c, :], identity=c_ident),
                     reads=["sel%d" % (ex % 2), "ident"], writes=["psb"], inc=(c == 7))
            P.op("act", lambda e: e.activation(out=SELT[:, ex % 2, :], in_=PSB, func=AF.Copy),
                 reads=["psb"], writes=["selt%d" % (ex % 2)])

        unit_tab = {}
        ld_state = {"next": 0, "consumed": -1}

        def unit_src(u):
            ex, j = divmod(u, 6)
            hh = j % 2
            if j < 2:
                return (w_gate[ex][hh * 1024:(hh + 1) * 1024, :].rearrange("(c p) f -> p c f", p=128), "p (c f) -> p c f", dict(c=8))
            if j < 4:
                return (w_up[ex][hh * 1024:(hh + 1) * 1024, :].rearrange("(c p) f -> p c f", p=128), "p (c f) -> p c f", dict(c=8))
            return (w_down[ex][hh * 256:(hh + 1) * 256, :].rearrange("(c p) d -> p c d", p=128), "p (c d) -> p c d", dict(c=2))

        def pump():
            while ld_state["next"] < 6 * NEXP and ld_state["next"] - NUNIT <= ld_state["consumed"]:
                u = ld_state["next"]
                src, view, kw = unit_src(u)
                unit_tab[u] = load_unit(src, view, kw)
                ld_state["next"] += 1

        def units_of(ex):
            return [unit_tab[6 * ex + j] for j in range(6)]

        def exp_A(ex, units):
            xg = XGB[ex % 2]
            for m, pb in ((0, 2), (1, 3)):
                for dc in range(16):
                    wv, wk = units[2 * m + dc // 8]
                    P.op("pe", lambda e, dc=dc, wv=wv, pb=pb: e.matmul(psf[pb], lhsT=xg[:, dc, :], rhs=wv[:, dc % 8, :],
                                                                     start=(dc == 0), stop=(dc == 15)),
                         reads=["xg%d" % (ex % 2), wk], writes=["psf%d" % pb], inc=(dc == 15))
            P.op("act", lambda e: e.activation(out=SILT, in_=psf[2], func=AF.Silu), reads=["psf2"], writes=["silt"])
            P.op("dve", lambda e: e.tensor_tensor(out=HROW, in0=SILT, in1=psf[3], op=ALU.mult),
                 reads=["silt", "psf3"], writes=["hrow"])

        def exp_B(ex):
            for fc in range(4):
                P.op("pe", lambda e, fc=fc: e.transpose(out=PSB[:, fc * 128:(fc + 1) * 128], in_=HROW[:, fc * 128:(fc + 1) * 128],
                                                       identity=c_ident),
                     reads=["hrow", "ident"], writes=["psb"], inc=(fc == 3))
            P.op("act", lambda e: e.activation(out=HTE, in_=PSB[:, 0:512].rearrange("p (c r) -> p c r", c=4), func=AF.Copy),
                 reads=["psb"], writes=["hte"])

        def exp_C(ex, units):
            el = ex % 2
            for ds in range(4):
                b = bankA()
                ps = psf[b]
                for fc in range(4):
                    wv, wk = units[4 + fc // 2]
                    P.op("pe", lambda e, fc=fc, ds=ds, ps=ps, wv=wv: e.matmul(
                        ps, lhsT=HTE[:, fc, :], rhs=wv[:, fc % 2, ds * 512:(ds + 1) * 512], start=(fc == 0), stop=(fc == 3)),
                        reads=["hte", wk], writes=["psf%d" % b], inc=(fc == 3))
                if ds % 2 == 0:
                    P.op("act", lambda e, ps=ps, ds=ds: e.activation(out=YG[:, el, ds * 512:(ds + 1) * 512], in_=ps, func=AF.Copy),
                         reads=["psf%d" % b], writes=["yg%d" % el])
                else:
                    P.op("dve", lambda e, ps=ps, ds=ds: e.tensor_copy(out=YG[:, el, ds * 512:(ds + 1) * 512], in_=ps),
                         reads=["psf%d" % b], writes=["yg%d" % el])

        def scatter_pair():
            for c in range(8):
                for ds in range(4):
                    b = 4 + sc_i[0] % 2
                    sc_i[0] += 1
                    ps = psf[b]
                    for el in range(2):
                        P.op("pe", lambda e, ps=ps, el=el, c=c, ds=ds: e.matmul(
                            ps, lhsT=SELT[:, el, c * 128:(c + 1) * 128], rhs=YG[:, el, ds * 512:(ds + 1) * 512],
                            start=(el == 0), stop=(el == 1)),
                            reads=["selt%d" % el, "yg%d" % el], writes=["psf%d" % b], inc=(el == 1))
                    hs = H[:, c, ds * 512:(ds + 1) * 512]
                    P.op("dve", lambda e, hs=hs, ps=ps: e.tensor_tensor(out=hs, in0=hs, in1=ps, op=ALU.add),
                         reads=["psf%d" % b, "H%d" % c], writes=["H%d" % c])

        pump()
        prep_sel(0)
        prep_gather(0, 0)
        prep_gather(0, 1)
        prep_selt(0)
        for ex in range(NEXP):
            if ex + 1 < NEXP:
                prep_sel(ex + 1)
            exp_A(ex, units_of(ex))
            ld_state["consumed"] = 6 * ex + 3
            pump()
            if ex + 1 < NEXP:
                prep_gather(ex + 1, 0)
            exp_B(ex)
            exp_C(ex, units_of(ex))
            ld_state["consumed"] = 6 * ex + 5
            pump()
            if ex + 1 < NEXP:
                prep_gather(ex + 1, 1)
            if ex % 2 == 1:
                scatter_pair()
            if ex + 1 < NEXP:
                prep_selt(ex + 1)

        P.barrier()
        P.dma("sp", c_g, g3.partition_broadcast(128), writes=["g"])
        OST = [A32(100 + 8 * r, 2048) for r in range(2)]
        JUNK3 = A16(116, 2048)
        for tc in range(8):
            s = tc % 2
            st = c_st[:, 40 + 4 * s:44 + 4 * s]
            P.op("act", lambda e, tc=tc, st=st: e.activation(out=JUNK3, in_=H[:, tc, :], func=AF.Square, accum_out=st[:, 0:1]),
                 reads=["H%d" % tc], writes=["junk3", "f_ss%d" % s])
            P.op("act", lambda e, st=st: e.activation(out=st[:, 1:2], in_=st[:, 0:1], func=AF.Sqrt, bias=c_eps, scale=1.0 / D),
                 reads=["f_ss%d" % s, "eps"], writes=["f_rms%d" % s])
            P.op("dve", lambda e, st=st: e.reciprocal(out=st[:, 2:3], in_=st[:, 1:2]), reads=["f_rms%d" % s], writes=["f_rstd%d" % s])
            P.op("dve", lambda e, tc=tc, st=st, s=s: e.scalar_tensor_tensor(
                out=OST[s], in0=H[:, tc, :], scalar=st[:, 2:3], in1=c_g, op0=ALU.mult, op1=ALU.mult),
                reads=["H%d" % tc, "f_rstd%d" % s, "g"], writes=["ost%d" % s])
            P.dma("sp", out_d[tc * 128:(tc + 1) * 128, :], OST[s], reads=["ost%d" % s], writes=["outd"])

        P.barrier(force=True)

        with nc.Block() as block:
            @block.tensor
            def _(e):
                for f in P.ops["pe"]:
                    f()

            @block.scalar
            def _(e):
                for f in P.ops["act"]:
                    f()

            @block.vector
            def _(e):
                for f in P.ops["dve"]:
                    f()

            @block.gpsimd
            def _(e):
                for f in P.ops["pool"]:
                    f()

            @block.sync
            def _(e):
                for f in P.ops["sp"]:
                    f()
    return nc


DBG_STAGE = int(os.environ.get("KDBG", "0"))
KPART = os.environ.get("KPART", "")


def _core_layout(core):
    b = core // 2
    own = A_BLOCKS if core % 2 == 0 else B_BLOCKS
    oth = B_BLOCKS if core % 2 == 0 else A_BLOCKS
    return b, own, oth


def make_in_maps(inputs):
    x = np.ascontiguousarray(inputs["x"], dtype=np.float32)
    f = lambda a: np.ascontiguousarray(np.asarray(a), dtype=np.float32)
    w_rt = np.concatenate([f(inputs["w_coarse"][0]), f(inputs["w_fine"][0]).transpose(1, 0, 2).reshape(D, 32)], axis=1)
    b_rt = np.concatenate([f(inputs["b_coarse"][0]).reshape(4), f(inputs["b_fine"][0]).reshape(32)]).reshape(1, 36)
    lamv = np.concatenate([f(inputs["lambda_q1"][0]), f(inputs["lambda_k1"][0]),
                           f(inputs["lambda_q2"][0]), f(inputs["lambda_k2"][0])]).reshape(1, 256)
    shared = {
        "g1": f(inputs["norm1_g"]).reshape(1, D), "g2": f(inputs["norm2_g"]).reshape(1, D),
        "g3": f(inputs["final_norm_g"]).reshape(1, D),
        "w_in": f(inputs["w_in"][0]), "lamv": lamv, "subg": f(inputs["subln_g"]).reshape(1, 128),
        "pool_w": f(inputs["pool_w"][0]), "pool_scale": np.ascontiguousarray(f(inputs["pool_scale"]).reshape(8, 128).T),
        "w_out": f(inputs["w_out"][0]), "w_rt": np.ascontiguousarray(w_rt), "b_rt": b_rt,
        "w_gate": f(inputs["w_gate"][0]), "w_up": f(inputs["w_up"][0]), "w_down": f(inputs["w_down"][0]),
        "ident": np.eye(128, dtype=np.float32),
        "iota": np.ascontiguousarray(np.broadcast_to(np.arange(128, dtype=np.float32)[None, :], (128, 128))),
        "ustr": np.triu(np.ones((128, 128), dtype=np.float32), 1),
        "trimask": np.triu(np.ones((128, 128), dtype=np.float32)),
    }
    slopes = np.exp2(-8.0 * np.arange(1, 9, dtype=np.float64) / 8.0)
    maps = []
    for core in range(NCORES):
        b, own, oth = _core_layout(core)
        slots = own + oth
        xa = np.zeros((17 * 128, D), dtype=np.float32)
        for si, blk in enumerate(slots):
            xa[si * 128:(si + 1) * 128] = x[b, blk * 128:(blk + 1) * 128]
        for i, blk in enumerate(own):
            if blk > 0:
                xa[16 * 128 + i * 16:16 * 128 + (i + 1) * 16] = x[b, blk * 128 - 16:blk * 128]
        xo = np.ascontiguousarray(xa[0:1024])
        ab = np.zeros((128, 8, 16, 8), dtype=np.float32)
        kk = np.arange(128, dtype=np.float64)
        for i in range(8):
            ref = own[i] * 128 + 64
            for j in range(16):
                if slots[j] > own[i]:
                    ab[:, i, j, :] = NEG
                else:
                    ab[:, i, j, :] = (slopes[None, :] * (slots[j] * 128 + kk[:, None] - ref)).astype(np.float32)
        pf = np.ones((128, 4, 8, 16), dtype=np.float32)
        for gi, w in enumerate((2, 4, 8, 16)):
            for i, blk in enumerate(own):
                pos = blk * 128 + np.arange(16)
                cnt = np.minimum(pos + 1, w).astype(np.float32)
                pf[:, gi, i, :] = (w / cnt)[None, :]
        m = dict(shared)
        m["xall"] = xa
        m["xown"] = xo
        m["abias"] = ab.reshape(128, 1024)
        m["poolfix"] = pf.reshape(128, 512)
        maps.append(m)
    return maps


def kernel(**inputs):
    nc = build_nc()
    in_maps = make_in_maps(inputs)
    res = run_bass_kernel_spmd(nc, in_maps, core_ids=list(range(NCORES)))
    out = np.zeros((4, S, D), dtype=np.float32)
    for core in range(NCORES):
        b, own, _ = _core_layout(core)
        o = res.results[core]["out"]
        for i, blk in enumerate(own):
            out[b, blk * 128:(blk + 1) * 128] = o[i * 128:(i + 1) * 128]
    return out
```
